# Optimizing a Trainium2 kernel written in Bass

```python
import math
import jax, jax.numpy as jnp
from jax import lax
import numpy as np

D_MODEL = 2048
BATCH = 2
SEQ = 16384
DEPTH = 2

N_EVEN = (DEPTH + 1) // 2
N_ODD = DEPTH // 2

POOL_WIDTH = D_MODEL // 2
POOL_WINDOWS = (2, 4, 8, 16)
POOL_GROUP = POOL_WIDTH // len(POOL_WINDOWS)
GMLP_WIDTH = D_MODEL // 2
GMLP_CHUNK = 128
GMLP_GROUPS = 8
GMLP_GROUP_DIM = GMLP_WIDTH // GMLP_GROUPS
IN_EVEN_WIDTH = POOL_WIDTH + 2 * GMLP_WIDTH

DIFF_HEAD_DIM = 64
DIFF_HEADS = D_MODEL // (2 * DIFF_HEAD_DIM)
DIFF_V_DIM = 2 * DIFF_HEAD_DIM
Q_BLOCK = 128
ROPE_THETA = 10000.0

D_FF = 5632
N_EXPERTS = 8
TOP_K = 2
EPS = 1e-5

kernel_name = "hybrid_pool_gmlp_diffattn_moe"


def rms_norm(x, g):
    xf = x.astype(jnp.float32)
    y = xf * lax.rsqrt(jnp.mean(xf * xf, axis=-1, keepdims=True) + EPS)
    return (y * g.astype(jnp.float32)).astype(x.dtype)


def layer_norm(x, g, b):
    xf = x.astype(jnp.float32)
    mu = jnp.mean(xf, axis=-1, keepdims=True)
    xc = xf - mu
    var = jnp.mean(xc * xc, axis=-1, keepdims=True)
    y = xc * lax.rsqrt(var + EPS) * g.astype(jnp.float32) + b.astype(jnp.float32)
    return y.astype(x.dtype)


def swiglu(h, w_gate, w_up, w_down):
    return (jax.nn.silu(h @ w_gate) * (h @ w_up)) @ w_down


def causal_pool_mixer(z, w_pool, pool_scale):
    S_ = z.shape[1]
    zf = z.astype(jnp.float32)
    cnt_base = jnp.arange(S_) + 1
    outs = []
    for g, w in enumerate(POOL_WINDOWS):
        zg = zf[..., g * POOL_GROUP:(g + 1) * POOL_GROUP]
        c = jnp.cumsum(zg, axis=1)
        lower = jnp.pad(c[:, :S_ - w], ((0, 0), (w, 0), (0, 0)))
        cnt = jnp.minimum(cnt_base, w).astype(jnp.float32)[None, :, None]
        pooled = ((c - lower) / cnt - zg).astype(z.dtype)
        outs.append(jnp.einsum('bsc,cd->bsd', pooled, w_pool[g]))
    return jnp.concatenate(outs, axis=-1) * pool_scale


def chunked_spatial_gating(z, ln_g, ln_b, w_spatial, b_spatial):
    a = jax.nn.gelu(z, approximate=False)
    u, v = jnp.split(a, 2, axis=-1)
    v = layer_norm(v, ln_g, ln_b)
    B_, S_, _ = v.shape
    v = v.reshape(B_, S_ // GMLP_CHUNK, GMLP_CHUNK, GMLP_GROUPS, GMLP_GROUP_DIM)
    mask = jnp.tril(jnp.ones((GMLP_CHUNK, GMLP_CHUNK), dtype=bool))
    w = jnp.where(mask[None], w_spatial, 0).astype(v.dtype)
    mixed = jnp.einsum('gts,bnsgc->bntgc', w, v) + b_spatial.T[None, None, :, :, None]
    return u * mixed.reshape(B_, S_, GMLP_WIDTH)


def rotary(x, cos, sin):
    x1, x2 = jnp.split(x, 2, axis=-1)
    return jnp.concatenate([x1 * cos - x2 * sin, x2 * cos + x1 * sin], axis=-1)


def diff_attention(h, positions, w_qkv, lam_q1, lam_k1, lam_q2, lam_k2, subln_g, w_o, lambda_init):
    B_, S_, _ = h.shape
    q, k, v = jnp.split(h @ w_qkv, 3, axis=-1)
    q = q.reshape(B_, S_, DIFF_HEADS, 2, DIFF_HEAD_DIM)
    k = k.reshape(B_, S_, DIFF_HEADS, 2, DIFF_HEAD_DIM)
    v = v.reshape(B_, S_, DIFF_HEADS, DIFF_V_DIM)
    inv_freq = ROPE_THETA ** (-jnp.arange(0, DIFF_HEAD_DIM, 2, dtype=jnp.float32) / DIFF_HEAD_DIM)
    ang = positions.astype(jnp.float32)[..., None] * inv_freq
    cos = jnp.cos(ang)[:, :, None, None, :].astype(h.dtype)
    sin = jnp.sin(ang)[:, :, None, None, :].astype(h.dtype)
    q = rotary(q, cos, sin) * (DIFF_HEAD_DIM ** -0.5)
    k = rotary(k, cos, sin)
    lam = (jnp.exp(jnp.sum((lam_q1 * lam_k1).astype(jnp.float32)))
           - jnp.exp(jnp.sum((lam_q2 * lam_k2).astype(jnp.float32)))
           + lambda_init)
    n_blk = S_ // Q_BLOCK
    q_blocks = q.reshape(B_, n_blk, Q_BLOCK, DIFF_HEADS, 2, DIFF_HEAD_DIM).transpose(1, 0, 2, 3, 4, 5)
    key_pos = jnp.arange(S_)

    def one_block(args):
        qb, i = args
        s = jnp.einsum('bqhcd,bkhcd->bhcqk', qb, k).astype(jnp.float32)
        q_pos = i * Q_BLOCK + jnp.arange(Q_BLOCK)
        causal = key_pos[None, :] <= q_pos[:, None]
        s = jnp.where(causal, s, -jnp.inf)
        p = jax.nn.softmax(s, axis=-1)
        att = p[:, :, 0] - lam * p[:, :, 1]
        return jnp.einsum('bhqk,bkhe->bqhe', att.astype(v.dtype), v)

    o = lax.map(one_block, (q_blocks, jnp.arange(n_blk)))
    o = o.transpose(1, 0, 2, 3, 4).reshape(B_, S_, DIFF_HEADS, DIFF_V_DIM)
    o = rms_norm(o, subln_g) * (1.0 - lambda_init)
    return o.reshape(B_, S_, DIFF_HEADS * DIFF_V_DIM) @ w_o


def moe_swiglu(h, w_router, we_gate, we_up, we_down):
    logits = (h @ w_router).astype(jnp.float32)
    top_v, top_i = lax.top_k(logits, TOP_K)
    top_w = jax.nn.softmax(top_v, axis=-1)
    gates = jnp.sum(jax.nn.one_hot(top_i, N_EXPERTS, dtype=jnp.float32) * top_w[..., None], axis=-2)
    y = jnp.zeros_like(h)
    for e in range(N_EXPERTS):
        y = y + gates[..., e:e + 1].astype(h.dtype) * swiglu(h, we_gate[e], we_up[e], we_down[e])
    return y


def setup_inputs(seed: int = 0) -> dict:
    key = jax.random.key(seed)
    ks = iter(jax.random.split(key, 40))
    f32 = jnp.float32

    def nrm(shape, scale):
        return jax.random.normal(next(ks), shape, f32) * scale

    def gain(shape):
        return 1.0 + 0.05 * jax.random.normal(next(ks), shape, f32)

    D = D_MODEL
    return {
        "x": jax.random.normal(next(ks), (BATCH, SEQ, D), f32),
        "positions": jnp.tile(jnp.arange(SEQ, dtype=jnp.int32)[None], (BATCH, 1)),
        "ev_norm_mix": gain((N_EVEN, D)),
        "ev_w_in": nrm((N_EVEN, D, IN_EVEN_WIDTH), D ** -0.5),
        "ev_w_pool": nrm((N_EVEN, len(POOL_WINDOWS), POOL_GROUP, POOL_GROUP), POOL_GROUP ** -0.5),
        "ev_pool_scale": gain((N_EVEN, POOL_WIDTH)),
        "ev_ln_g": gain((N_EVEN, GMLP_WIDTH)),
        "ev_ln_b": nrm((N_EVEN, GMLP_WIDTH), 0.02),
        "ev_w_spatial": nrm((N_EVEN, GMLP_GROUPS, GMLP_CHUNK, GMLP_CHUNK), GMLP_CHUNK ** -0.5),
        "ev_b_spatial": 1.0 + nrm((N_EVEN, GMLP_GROUPS, GMLP_CHUNK), 0.02),
        "ev_w_out": nrm((N_EVEN, POOL_WIDTH + GMLP_WIDTH, D), (POOL_WIDTH + GMLP_WIDTH) ** -0.5),
        "ev_norm_ffn": gain((N_EVEN, D)),
        "ev_w_gate": nrm((N_EVEN, D, D_FF), D ** -0.5),
        "ev_w_up": nrm((N_EVEN, D, D_FF), D ** -0.5),
        "ev_w_down": nrm((N_EVEN, D_FF, D), D_FF ** -0.5),
        "od_norm_attn": gain((N_ODD, D)),
        "od_w_qkv": nrm((N_ODD, D, 3 * D), D ** -0.5),
        "od_lam_q1": nrm((N_ODD, DIFF_HEAD_DIM), 0.1),
        "od_lam_k1": nrm((N_ODD, DIFF_HEAD_DIM), 0.1),
        "od_lam_q2": nrm((N_ODD, DIFF_HEAD_DIM), 0.1),
        "od_lam_k2": nrm((N_ODD, DIFF_HEAD_DIM), 0.1),
        "od_subln_g": gain((N_ODD, DIFF_V_DIM)),
        "od_w_o": nrm((N_ODD, D, D), D ** -0.5),
        "od_norm_moe": gain((N_ODD, D)),
        "od_w_router": nrm((N_ODD, D, N_EXPERTS), D ** -0.5),
        "od_we_gate": nrm((N_ODD, N_EXPERTS, D, D_FF), D ** -0.5),
        "od_we_up": nrm((N_ODD, N_EXPERTS, D, D_FF), D ** -0.5),
        "od_we_down": nrm((N_ODD, N_EXPERTS, D_FF, D), D_FF ** -0.5),
        "final_norm": gain((D,)),
    }


def reference(x, positions,
              ev_norm_mix, ev_w_in, ev_w_pool, ev_pool_scale, ev_ln_g, ev_ln_b,
              ev_w_spatial, ev_b_spatial, ev_w_out, ev_norm_ffn, ev_w_gate, ev_w_up, ev_w_down,
              od_norm_attn, od_w_qkv, od_lam_q1, od_lam_k1, od_lam_q2, od_lam_k2, od_subln_g,
              od_w_o, od_norm_moe, od_w_router, od_we_gate, od_we_up, od_we_down,
              final_norm):
    for i in range(DEPTH):
        j = i // 2
        if i % 2 == 0:
            h = rms_norm(x, ev_norm_mix[j])
            z = h @ ev_w_in[j]
            y_pool = causal_pool_mixer(z[..., :POOL_WIDTH], ev_w_pool[j], ev_pool_scale[j])
            y_gate = chunked_spatial_gating(z[..., POOL_WIDTH:], ev_ln_g[j], ev_ln_b[j],
                                            ev_w_spatial[j], ev_b_spatial[j])
            x = x + jnp.concatenate([y_pool, y_gate], axis=-1) @ ev_w_out[j]
            x = x + swiglu(rms_norm(x, ev_norm_ffn[j]), ev_w_gate[j], ev_w_up[j], ev_w_down[j])
        else:
            lambda_init = 0.8 - 0.6 * math.exp(-0.3 * i)
            h = rms_norm(x, od_norm_attn[j])
            x = x + diff_attention(h, positions, od_w_qkv[j], od_lam_q1[j], od_lam_k1[j],
                                   od_lam_q2[j], od_lam_k2[j], od_subln_g[j], od_w_o[j], lambda_init)
            x = x + moe_swiglu(rms_norm(x, od_norm_moe[j]), od_w_router[j],
                               od_we_gate[j], od_we_up[j], od_we_down[j])
    return rms_norm(x, final_norm)
```

```python
import contextlib
import math
import numpy as np
import ml_dtypes
import concourse.bass as bass
import concourse.mybir as mybir
from concourse.bass_utils import run_bass_kernel_spmd

F32, BF16, I32 = mybir.dt.float32, mybir.dt.bfloat16, mybir.dt.int32
AF = mybir.ActivationFunctionType
ALU = mybir.AluOpType
AX = mybir.AxisListType

D = 2048
KC = 16
T = 512
NCORES = 8
EPS = 1e-5
PI = math.pi


class Sess:
    ENG = {"pe": "tensor", "act": "scalar", "dve": "vector", "pool": "gpsimd", "sp": "sync"}

    def __init__(self, nc, ndma=10):
        self.nc, self.ndma = nc, ndma
        self.stack = contextlib.ExitStack()
        en = self.stack.enter_context
        self.sem = {k: en(nc.semaphore("p_" + k)) for k in ("pe", "act", "dve", "pool")}
        self.dsem = {k: [en(nc.semaphore("d_%s%d" % (k, q))) for q in range(ndma)] for k in ("sp", "pool")}
        self.ccsem = en(nc.semaphore("ccs"))
        self.cccnt = 0
        self.dtot = {}
        self.dnext = {"sp": 0, "pool": 0}
        self.cnt = {"pe": 0, "act": 0, "dve": 0, "pool": 0}
        self.waited = {k: {} for k in self.ENG}
        self.engobj = {k: getattr(nc, v) for k, v in self.ENG.items()}

    def close(self):
        self.stack.close()


class Prog:
    def __init__(self, sess, prefix):
        self.S = sess
        self.nc = sess.nc
        self.prefix = prefix
        self.ops = []
        self.stack = contextlib.ExitStack()

    def sb(self, name, shape, dt):
        return self.stack.enter_context(self.nc.sbuf_tensor(self.prefix + name, list(shape), dt))

    def ps(self, name, shape, dt=F32):
        return self.stack.enter_context(self.nc.psum_tensor(self.prefix + name, list(shape), dt))

    def add(self, eng, fn, r=(), w=(), dma=False):
        self.ops.append([eng, fn, tuple(r), tuple(w), dma])

    def dma(self, eng, out, in_, r=(), w=()):
        self.add(eng, lambda e: e.dma_start(out=out, in_=in_), r, w, dma=True)

    def cc(self, fn, r=(), w=()):
        self.add("pool", fn, r, w, dma="cc")

    def mm(self, out, lhsT, rhs, start, stop, r=(), w=()):
        self.add("pe", lambda e: e.matmul(out, lhsT, rhs, start=start, stop=stop), r, w)

    def emit(self):
        S, ops = self.S, self.ops
        n = len(ops)
        last_w, readers = {}, {}
        deps = [None] * n
        for i, (eng, fn, r, w, dma) in enumerate(ops):
            d = set()
            for b in r:
                if b in last_w:
                    d.add(last_w[b])
            for b in w:
                if b in last_w:
                    d.add(last_w[b])
                d.update(readers.get(b, ()))
            d.discard(i)
            if eng == "pe":
                d = {j for j in d if not (ops[j][0] == "pe" and not ops[j][4])}
            deps[i] = d
            for b in r:
                readers.setdefault(b, []).append(i)
            for b in w:
                last_w[b] = i
                readers[b] = []
        marked = [False] * n
        for i in range(n):
            for j in deps[i]:
                marked[j] = True
        for k in ("pe", "act", "dve", "pool"):
            for i in range(n - 1, -1, -1):
                if ops[i][0] == k and not ops[i][4]:
                    marked[i] = True
                    break
        token = [None] * n

        def do_wait(eng, s, v):
            if S.waited[eng].get(s.name, 0) >= v:
                return
            S.engobj[eng].wait_ge(s, v)
            S.waited[eng][s.name] = v

        for i, (eng, fn, r, w, dma) in enumerate(ops):
            for j in sorted(deps[i]):
                s, v = token[j]
                do_wait(eng, s, v)
            if dma == "cc":
                ins = fn(S.engobj[eng])
                ins.then_inc(S.ccsem)
                S.cccnt += 1
                token[i] = (S.ccsem, S.cccnt)
            elif dma:
                q = S.dnext[eng]
                S.dnext[eng] = (q + 1) % S.ndma
                s = S.dsem[eng][q]
                prev = S.dtot.get(s.name, 0)
                if prev:
                    do_wait(eng, s, prev)
                ins = fn(S.engobj[eng])
                ins.then_inc(s, 16)
                S.dtot[s.name] = prev + 16
                token[i] = (s, prev + 16)
            else:
                ins = fn(S.engobj[eng])
                if marked[i]:
                    S.cnt[eng] += 1
                    ins.then_inc(S.sem[eng], 1)
                    token[i] = (S.sem[eng], S.cnt[eng])
        for eng in S.ENG:
            for k in S.sem:
                if S.cnt[k]:
                    do_wait(eng, S.sem[k], S.cnt[k])
            for k in S.dsem:
                for s in S.dsem[k]:
                    if S.dtot.get(s.name, 0):
                        do_wait(eng, s, S.dtot[s.name])
            if S.cccnt:
                do_wait(eng, S.ccsem, S.cccnt)
        self.stack.close()


def _bc(ap, shape):
    return ap.to_broadcast(list(shape))


class Common:
    def __init__(self, P, nc, nw=4):
        self.P, self.nc = P, nc
        self.nw = nw
        self.wr = [P.sb("wring%d" % i, [128, 2048], BF16) for i in range(nw)]
        self.wi = 0
        self.bank = [P.ps("bank%d" % i, [128, 512], F32) for i in range(8)]
        self.bi = 0
        self.xt = P.sb("xt", [128, KC, T], F32)
        self.ht = P.sb("ht", [128, KC, T], BF16)
        self.rstd = P.sb("rstd", [128, T], F32)
        self.ones = P.sb("ones", [128, 128], BF16)
        P.add("dve", lambda e: e.memset(self.ones[:], 1.0), w=["ones"])

    def wload(self, src_ap, nelem):
        i = self.wi
        self.wi = (i + 1) % self.nw
        t = self.wr[i]
        self.P.dma("pool", t[:, 0:nelem], src_ap, w=[("w", i)])
        return t, ("w", i)

    def nb(self):
        i = self.bi
        self.bi = (i + 1) % 8
        return self.bank[i], ("ps", i)

    def rmsnorm(self, gcol, tag):
        P, xt, ht = self.P, self.xt, self.ht
        P.add("act", lambda e: e.activation(out=ht[:], in_=xt[:], func=AF.Square), r=["xt"], w=["ht"])
        bk, bkey = self.nb()
        for kc in range(KC):
            P.mm(bk[:, :], self.ones[:, :], ht[:, kc, :], kc == 0, kc == KC - 1, r=["ht", "ones"], w=[bkey])
        rs = self.rstd
        P.add("act", lambda e: e.activation(out=rs[:], in_=bk[:, :], func=AF.Sqrt, bias=EPS_AP[0][:, 0:1], scale=1.0 / D),
              r=[bkey, "eps"], w=["rstd"])
        P.add("dve", lambda e: e.reciprocal(rs[:], rs[:]), r=["rstd"], w=["rstd"])
        for kc in range(KC):
            P.add("dve", lambda e, kc=kc: e.scalar_tensor_tensor(out=ht[:, kc, :], in0=xt[:, kc, :], scalar=gcol[:, kc:kc + 1],
                                                                   in1=rs[:], op0=ALU.mult, op1=ALU.mult),
                  r=["xt", "rstd", "consts"], w=["ht"])


EPS_AP = [None]


def _mk_eps(P):
    t = P.sb("epsc", [128, 1], F32)
    P.add("dve", lambda e: e.memset(t[:], EPS), w=["eps"])
    EPS_AP[0] = t


def lhs_matmul_group(P, C, wt, wkey, nk, rhs_fn, rkeys, bk=None, bkey=None, first=True, last=True, kbase=0, ncols=T):
    if bk is None:
        bk, bkey = C.nb()
    wv = wt[:, 0:nk * 128].rearrange("p (k m) -> p k m", m=128)
    for k in range(nk):
        P.mm(bk[:, 0:ncols], wv[:, k, :], rhs_fn(kbase + k), first and k == 0, last and k == nk - 1,
             r=[wkey] + list(rkeys), w=[bkey])
    return bk, bkey


def phase_a(sess, A, NTOK, NP):
    NFF = 11 * NP
    NT = NTOK // T
    nc = sess.nc
    NB = NTOK // T
    xT, xh, pos, invcnt, cols, lng, lnb, bsp, wsT, tri, wpool, rotP = [A[k] for k in
        ("xT", "xh", "pos", "invcnt", "cols", "lng", "lnb", "bsp", "wsT", "tri", "wpool", "rotP")]
    w_inA, w_inV, w_out, w_gu, w_dn, w_qk, w_v = [A[k] for k in ("w_inA", "w_inV", "w_out", "w_gu", "w_dn", "w_qk", "w_v")]
    x2T, qT, kT, vtok = A["x2s"], A["qs"], A["ks"], A["vs"]
    P = Prog(sess, "a_")
    _mk_eps(P)
    C = Common(P, nc)
    xt, ht = C.xt, C.ht
    cst = P.sb("cst", [128, 64], F32)
    P.dma("sp", cst[:], cols[:, :], w=["consts"])
    G_MIX, PSC, G_FFN, G_ATT, IFQ, SGN = 0, 16, 24, 40, 56, 57
    lng_bc = P.sb("lng_bc", [128, 1024], F32)
    lnb_bc = P.sb("lnb_bc", [128, 1024], F32)
    bsp_bc = P.sb("bsp_bc", [128, 8, 128], F32)
    icn_bc = P.sb("icn_bc", [128, NB, 4, 16], F32)
    P.dma("sp", lng_bc[:], lng[0, :].partition_broadcast(128), w=["consts"])
    P.dma("sp", lnb_bc[:], lnb[0, :].partition_broadcast(128), w=["consts"])
    P.dma("sp", bsp_bc[:].rearrange("p g t -> p (g t)"), bsp[0, :].partition_broadcast(128), w=["consts"])
    P.dma("sp", icn_bc[:].rearrange("p b g t -> p (b g t)"), invcnt[0, :].partition_broadcast(128), w=["consts"])
    wsf = P.sb("wsf", [128, 8, 128], F32)
    trif = P.sb("trif", [128, 128], F32)
    wsm = P.sb("wsm", [128, 8, 128], BF16)
    P.dma("sp", wsf[:].rearrange("p g t -> p (g t)"), wsT[:, :], w=["wsf"])
    P.dma("sp", trif[:], tri[:, :], w=["trif"])
    P.add("dve", lambda e: e.tensor_tensor(out=wsm[:], in0=wsf[:], in1=trif[:].unsqueeze(1).to_broadcast([128, 8, 128]), op=ALU.mult),
          r=["wsf", "trif"], w=["consts"])
    wpl = P.sb("wpl", [128, 16, 128], BF16)
    P.dma("pool", wpl[:].rearrange("p a m -> p (a m)"), wpool[:, :], w=["consts"])
    rot = P.sb("rot", [128, 128], BF16)
    P.dma("pool", rot[:], rotP[:, :], w=["consts"])

    scr = P.sb("scr", [128, max(44, 32), T], BF16)
    PO, YP, YG, UU = 0, 8, 16, 24
    zp = P.sb("zp", [128, 8, 16 + T], F32)
    pa = P.sb("pa", [128, 16 + T], F32)
    pb = P.sb("pb", [128, 16 + T], F32)
    vg = P.sb("vg", [128, 1024], F32)
    vln = P.sb("vln", [128, 1024], BF16)
    st = P.sb("st", [128, 8], F32)
    tmpf = P.sb("tmpf", [128, T], F32)
    tmpg = P.sb("tmpg", [128, T], F32)
    posi = P.sb("posi", [128, T], I32)
    ang = P.sb("ang", [128, T], F32)
    cosT = P.sb("cosT", [128, T], F32)
    sinT = P.sb("sinT", [128, T], F32)
    qsb = P.sb("qsb", [128, T], BF16)
    qo = [P.sb("qo%d" % i, [128, T], BF16) for i in range(2)]
    vsb = P.sb("vsb", [128, 4, D], BF16)
    negpi = P.sb("negpi", [128, 1], F32)
    P.add("dve", lambda e: e.memset(negpi[:], -PI), w=["consts"])

    xT_v = xT.rearrange("(k p) t -> p k t", p=128)
    x2T_v = x2T.rearrange("(k p) t -> p k t", p=128)

    xhs = P.sb("xhs", [128, KC, 16], F32)
    hh = P.sb("hh", [128, KC, 16], BF16)
    rsh = P.sb("rsh", [128, 16], F32)
    xh_v = xh.rearrange("b (k p) t -> b p k t", p=128)

    for it in range(NT):
        t0 = it * T
        P.dma("sp", xt[:], xT_v[:, :, t0:t0 + T], w=["xt"])
        C.rmsnorm(cst[:, G_MIX:G_MIX + 16], "m")
        P.dma("sp", xhs[:], xh_v[it], w=["xhs"])
        P.add("act", lambda e: e.activation(out=hh[:], in_=xhs[:], func=AF.Square), r=["xhs"], w=["hh"])
        bk, bkey = C.nb()
        for kc in range(KC):
            P.mm(bk[:, 0:16], C.ones[:, :], hh[:, kc, :], kc == 0, kc == KC - 1, r=["hh", "ones"], w=[bkey])
        P.add("act", lambda e, bk=bk: e.activation(out=rsh[:], in_=bk[:, 0:16], func=AF.Sqrt, bias=EPS_AP[0][:, 0:1], scale=1.0 / D),
              r=[bkey, "eps"], w=["rsh"])
        P.add("dve", lambda e: e.reciprocal(rsh[:], rsh[:]), r=["rsh"], w=["rsh"])
        for kc in range(KC):
            P.add("dve", lambda e, kc=kc: e.scalar_tensor_tensor(out=hh[:, kc, :], in0=xhs[:, kc, :], scalar=cst[:, G_MIX + kc:G_MIX + kc + 1],
                                                                   in1=rsh[:], op0=ALU.mult, op1=ALU.mult), r=["xhs", "rsh", "consts"], w=["hh"])
        for j in range(8):
            wt, wk = C.wload(w_inA[j], 2048)
            bk, bkey = lhs_matmul_group(P, C, wt, wk, KC, lambda k: ht[:, k, :], ["ht"])
            P.add("act", lambda e, j=j, bk=bk: e.activation(out=zp[:, j, 16:16 + T], in_=bk[:, :], func=AF.Copy), r=[bkey], w=["zp"])
            bk, bkey = lhs_matmul_group(P, C, wt, wk, KC, lambda k: hh[:, k, :], ["hh"], ncols=16)
            P.add("act", lambda e, j=j, bk=bk: e.activation(out=zp[:, j, 0:16], in_=bk[:, 0:16], func=AF.Copy), r=[bkey], w=["zp"])
        for j in range(8):
            wt, wk = C.wload(w_inA[8 + j], 2048)
            bk, bkey = lhs_matmul_group(P, C, wt, wk, KC, lambda k: ht[:, k, :], ["ht"])
            P.add("act", lambda e, j=j, bk=bk: e.activation(out=scr[:, UU + j, :], in_=bk[:, :], func=AF.Gelu),
                  r=[bkey], w=[("scr", UU + j)])
        for j in range(8):
            g = j // 2
            w = 2 << g
            z = zp[:, j, :]
            src, bufs, sh = z, [pa, pb], 1
            for stp in range(g + 1):
                dst = bufs[stp % 2]
                s_ = src
                P.add("dve", lambda e, dst=dst, s_=s_, sh=sh: e.tensor_tensor(out=dst[:, sh:16 + T], in0=s_[:, sh:16 + T],
                                                                               in1=s_[:, 0:16 + T - sh], op=ALU.add),
                      r=["zp", "pa", "pb"], w=["pa", "pb"])
                src = dst[:, :]
                sh *= 2
            P.add("dve", lambda e, j=j, src=src, w=w: e.scalar_tensor_tensor(out=scr[:, PO + j, :], in0=src[:, 16:16 + T], scalar=1.0 / w,
                                                                             in1=zp[:, j, 16:16 + T], op0=ALU.mult, op1=ALU.subtract),
                  r=["zp", "pa", "pb"], w=[("scr", PO + j)])
            if True:
                P.add("dve", lambda e, src=src, g=g, it=it: e.tensor_tensor(out=tmpf[:, 0:16], in0=src[:, 16:32], in1=icn_bc[:, it, g, :], op=ALU.mult),
                      r=["pa", "pb", "consts"], w=["tmpf"])
                P.add("dve", lambda e, j=j: e.tensor_tensor(out=scr[:, PO + j, 0:16], in0=tmpf[:, 0:16], in1=zp[:, j, 16:32], op=ALU.subtract),
                      r=["tmpf", "zp"], w=[("scr", PO + j)])
        for g in range(4):
            for do in range(2):
                bk, bkey = C.nb()
                for ci in range(2):
                    P.mm(bk[:, :], wpl[:, (g * 2 + ci) * 2 + do, :], scr[:, PO + 2 * g + ci, :], ci == 0, ci == 1,
                         r=["consts", ("scr", PO + 2 * g + ci)], w=[bkey])
                P.add("act", lambda e, bk=bk, g=g, do=do: e.activation(out=scr[:, YP + 2 * g + do, :], in_=bk[:, :], func=AF.Copy,
                                                                        scale=cst[:, PSC + 2 * g + do:PSC + 2 * g + do + 1]),
                      r=[bkey, "consts"], w=[("scr", YP + 2 * g + do)])
        for s in range(4):
            for n in range(2):
                bks = None
                bk, bkey = C.nb()
                for q in range(4):
                    wt, wk = C.wload(w_inV[n * 4 + q], 2048)
                    wv = wt[:, :].rearrange("p (k m) -> p k m", m=512)
                    for kk in range(4):
                        kc = 4 * q + kk
                        P.mm(bk[:, :], ht[:, kc, s * 128:(s + 1) * 128], wv[:, kk, :], kc == 0, kc == KC - 1,
                             r=["ht", wk], w=[bkey])
                P.add("act", lambda e, bk=bk, n=n: e.activation(out=vg[:, n * 512:(n + 1) * 512], in_=bk[:, :], func=AF.Gelu),
                      r=[bkey], w=["vg"])
            P.add("dve", lambda e: e.memset(st[:], 0.0), w=["st"])
            P.add("dve", lambda e: e.reduce_sum(out=st[:, 0:1], in_=vg[:], axis=AX.X), r=["vg"], w=["st"])
            P.add("act", lambda e: e.activation(out=tmpg[:, :], in_=vg[:, 0:512], func=AF.Square, accum_out=st[:, 1:2]),
                  r=["vg"], w=["tmpg", "st"])
            P.add("act", lambda e: e.activation(out=tmpg[:, :], in_=vg[:, 512:1024], func=AF.Square, accum_out=st[:, 2:3]),
                  r=["vg"], w=["tmpg", "st"])
            P.add("dve", lambda e: e.tensor_scalar(out=st[:, 3:4], in0=st[:, 0:1], scalar1=1.0 / 1024, scalar2=None, op0=ALU.mult),
                  r=["st"], w=["st"])
            P.add("dve", lambda e: e.tensor_tensor(out=st[:, 4:5], in0=st[:, 1:2], in1=st[:, 2:3], op=ALU.add), r=["st"], w=["st"])
            P.add("dve", lambda e: e.tensor_tensor(out=st[:, 5:6], in0=st[:, 3:4], in1=st[:, 3:4], op=ALU.mult), r=["st"], w=["st"])
            P.add("dve", lambda e: e.scalar_tensor_tensor(out=st[:, 6:7], in0=st[:, 4:5], scalar=1.0 / 1024, in1=st[:, 5:6],
                                                          op0=ALU.mult, op1=ALU.subtract), r=["st"], w=["st"])
            P.add("act", lambda e: e.activation(out=st[:, 6:7], in_=st[:, 6:7], func=AF.Sqrt, bias=EPS_AP[0][:, 0:1], scale=1.0),
                  r=["st", "eps"], w=["st"])
            P.add("dve", lambda e: e.reciprocal(st[:, 6:7], st[:, 6:7]), r=["st"], w=["st"])
            P.add("dve", lambda e: e.scalar_tensor_tensor(out=st[:, 7:8], in0=st[:, 3:4], scalar=-1.0, in1=st[:, 6:7],
                                                          op0=ALU.mult, op1=ALU.mult), r=["st"], w=["st"])
            P.add("act", lambda e: e.activation(out=vg[:], in_=vg[:], func=AF.Identity, bias=st[:, 7:8], scale=st[:, 6:7]),
                  r=["vg", "st"], w=["vg"])
            P.add("dve", lambda e: e.tensor_tensor(out=vg[:], in0=vg[:], in1=lng_bc[:], op=ALU.mult), r=["vg", "consts"], w=["vg"])
            P.add("dve", lambda e: e.tensor_tensor(out=vln[:], in0=vg[:], in1=lnb_bc[:], op=ALU.add), r=["vg", "consts"], w=["vln"])
            for gh in range(2):
                bk, bkey = C.nb()
                for gg in range(4):
                    g = gh * 4 + gg
                    P.mm(bk[:, gg * 128:(gg + 1) * 128], vln[:, g * 128:(g + 1) * 128], wsm[:, g, :], True, True,
                         r=["vln", "consts"], w=[bkey])
                bkv = bk[:, :].rearrange("p (g t) -> p g t", t=128)
                tv = tmpf[:, :].rearrange("p (g t) -> p g t", t=128)
                P.add("dve", lambda e, bkv=bkv, tv=tv, gh=gh: e.tensor_tensor(out=tv, in0=bkv, in1=bsp_bc[:, gh * 4:gh * 4 + 4, :], op=ALU.add),
                      r=[bkey, "consts"], w=["tmpf"])
                P.add("dve", lambda e, tv=tv, gh=gh, s=s: e.tensor_tensor(out=scr[:, YG + gh * 4:YG + gh * 4 + 4, s * 128:(s + 1) * 128], in0=tv,
                                                                           in1=scr[:, UU + gh * 4:UU + gh * 4 + 4, s * 128:(s + 1) * 128], op=ALU.mult),
                      r=["tmpf"] + [("scr", UU + gh * 4 + x) for x in range(4)], w=[("scr", YG + gh * 4 + x) for x in range(4)])
        for fo in range(16):
            wt, wk = C.wload(w_out[fo], 2048)
            bk, bkey = lhs_matmul_group(P, C, wt, wk, KC, lambda k: scr[:, YP + k, :], [("scr", YP + x) for x in range(16)])
            P.add("dve", lambda e, fo=fo, bk=bk: e.tensor_tensor(out=xt[:, fo, :], in0=xt[:, fo, :], in1=bk[:, :], op=ALU.add),
                  r=["xt", bkey], w=["xt"])
        C.rmsnorm(cst[:, G_FFN:G_FFN + 16], "f")
        for j in range(NFF):
            wt, wk = C.wload(w_gu[2 * j], 2048)
            bg, bgk = lhs_matmul_group(P, C, wt, wk, KC, lambda k: ht[:, k, :], ["ht"])
            wt, wk = C.wload(w_gu[2 * j + 1], 2048)
            bu, buk = lhs_matmul_group(P, C, wt, wk, KC, lambda k: ht[:, k, :], ["ht"])
            P.add("act", lambda e, bg=bg: e.activation(out=tmpg[:], in_=bg[:, :], func=AF.Silu), r=[bgk], w=["tmpg"])
            P.add("dve", lambda e, bu=bu, j=j: e.tensor_tensor(out=scr[:, j, :], in0=tmpg[:], in1=bu[:, :], op=ALU.mult),
                  r=["tmpg", buk], w=[("scr", j)])
        for fo in range(16):
            bk, bkey = C.nb()
            for pc in range(NP):
                wt, wk = C.wload(w_dn[fo * NP + pc], 1408)
                lhs_matmul_group(P, C, wt, wk, 11, lambda k: scr[:, k, :], [("scr", x) for x in range(pc * 11, pc * 11 + 11)],
                                 bk=bk, bkey=bkey, first=(pc == 0), last=(pc == NP - 1), kbase=pc * 11)
            P.add("dve", lambda e, fo=fo, bk=bk: e.tensor_tensor(out=xt[:, fo, :], in0=xt[:, fo, :], in1=bk[:, :], op=ALU.add),
                  r=["xt", bkey], w=["xt"])
        P.dma("sp", x2T_v[:, :, t0:t0 + T], xt[:], r=["xt"])
        C.rmsnorm(cst[:, G_ATT:G_ATT + 16], "a")
        P.dma("sp", posi[:], pos[0, t0:t0 + T].partition_broadcast(128), w=["posi"])
        P.add("dve", lambda e: e.tensor_copy(out=ang[:], in_=posi[:]), r=["posi"], w=["ang"])
        P.add("dve", lambda e: e.tensor_scalar(out=ang[:], in0=ang[:], scalar1=cst[:, IFQ:IFQ + 1], scalar2=None, op0=ALU.mult),
              r=["ang", "consts"], w=["ang"])
        def sin_of(dst, shift):
            P.add("dve", lambda e: e.tensor_scalar(out=tmpf[:], in0=ang[:], scalar1=shift, scalar2=1.0 / (2 * PI), op0=ALU.add, op1=ALU.mult),
                  r=["ang"], w=["tmpf"])
            P.add("dve", lambda e: e.tensor_copy(out=posi[:], in_=tmpf[:]), r=["tmpf"], w=["posi"])
            P.add("dve", lambda e: e.tensor_copy(out=tmpf[:], in_=posi[:]), r=["posi"], w=["tmpf"])
            P.add("dve", lambda e: e.tensor_scalar(out=tmpg[:], in0=ang[:], scalar1=shift, scalar2=None, op0=ALU.add), r=["ang"], w=["tmpg"])
            P.add("dve", lambda e: e.scalar_tensor_tensor(out=tmpg[:], in0=tmpf[:], scalar=-2 * PI, in1=tmpg[:], op0=ALU.mult, op1=ALU.add),
                  r=["tmpf", "tmpg"], w=["tmpg"])
            P.add("dve", lambda e: e.tensor_scalar(out=tmpf[:], in0=tmpg[:], scalar1=-PI, scalar2=1e9, op0=ALU.add, op1=ALU.mult),
                  r=["tmpg"], w=["tmpf"])
            P.add("dve", lambda e: e.tensor_scalar(out=tmpf[:], in0=tmpf[:], scalar1=0.0, scalar2=1.0, op0=ALU.max, op1=ALU.min),
                  r=["tmpf"], w=["tmpf"])
            P.add("dve", lambda e: e.scalar_tensor_tensor(out=tmpg[:], in0=tmpf[:], scalar=-2 * PI, in1=tmpg[:], op0=ALU.mult, op1=ALU.add),
                  r=["tmpf", "tmpg"], w=["tmpg"])
            P.add("dve", lambda e: e.tensor_scalar(out=tmpg[:], in0=tmpg[:], scalar1=-PI, scalar2=PI, op0=ALU.max, op1=ALU.min),
                  r=["tmpg"], w=["tmpg"])
            P.add("act", lambda e: e.activation(out=dst[:], in_=tmpg[:], func=AF.Sin), r=["tmpg"], w=["sincos"])
        sin_of(sinT, 0.0)
        P.add("dve", lambda e: e.tensor_scalar(out=sinT[:], in0=sinT[:], scalar1=cst[:, SGN:SGN + 1], scalar2=None, op0=ALU.mult),
              r=["sincos", "consts"], w=["sincos"])
        sin_of(cosT, 0.5 * PI)
        for c in range(32):
            wt, wk = C.wload(w_qk[c], 2048)
            bk, bkey = lhs_matmul_group(P, C, wt, wk, KC, lambda k: ht[:, k, :], ["ht"])
            sc = 0.125 if c < 16 else 1.0
            P.add("act", lambda e, bk=bk, sc=sc: e.activation(out=qsb[:], in_=bk[:, :], func=AF.Copy, scale=sc), r=[bkey], w=["qsb"])
            b2, b2k = C.nb()
            P.mm(b2[:, :], rot[:, :], qsb[:, :], True, True, r=["qsb", "consts"], w=[b2k])
            P.add("dve", lambda e: e.tensor_tensor(out=tmpf[:], in0=qsb[:], in1=cosT[:], op=ALU.mult), r=["qsb", "sincos"], w=["tmpf"])
            P.add("dve", lambda e, b2=b2: e.tensor_tensor(out=tmpg[:], in0=b2[:, :], in1=sinT[:], op=ALU.mult), r=[b2k, "sincos"], w=["tmpg"])
            o = qo[c % 2]
            ok = ("qo", c % 2)
            P.add("dve", lambda e, o=o: e.tensor_tensor(out=o[:], in0=tmpf[:], in1=tmpg[:], op=ALU.add), r=["tmpf", "tmpg"], w=[ok])
            dst = (qT if c < 16 else kT)[(c % 16) * 128:(c % 16 + 1) * 128, t0:t0 + T]
            P.dma("sp", dst, o[:], r=[ok])
        for n in range(4):
            for s in range(4):
                bk, bkey = C.nb()
                for q in range(4):
                    wt, wk = C.wload(w_v[n * 4 + q], 2048)
                    wv = wt[:, :].rearrange("p (k m) -> p k m", m=512)
                    for kk in range(4):
                        kc = 4 * q + kk
                        P.mm(bk[:, :], ht[:, kc, s * 128:(s + 1) * 128], wv[:, kk, :], kc == 0, kc == KC - 1, r=["ht", wk], w=[bkey])
                P.add("act", lambda e, bk=bk, s=s, n=n: e.activation(out=vsb[:, s, n * 512:(n + 1) * 512], in_=bk[:, :], func=AF.Copy),
                      r=[bkey], w=["vsb"])
        P.dma("sp", vtok[t0:t0 + T, :].rearrange("(s p) d -> p s d", p=128), vsb[:], r=["vsb"])
    P.emit()


def phase_b(sess, A, NTOK, S, lambda_init):
    nc = sess.nc
    NB2 = NTOK // T // 2
    W2 = NB2 * T
    NKT = S // 128
    KPM = W2 // 128
    qs, ks, vs, kg, vg, os_ = A["qs"], A["ks"], A["vs"], A["kg"], A["vg"], A["os"]
    lamv, gsub, diag, mcol, ident = A["lamv"], A["gsub"], A["diag"], A["mcol"], A["ident"]
    P = Prog(sess, "b_")
    _mk_eps(P)
    P.cc(lambda e: e.collective_compute("AllGather", ALU.bypass, replica_groups=[list(range(NCORES))], ins=[ks[:, :]], outs=[kg[:, :]]),
         w=["kg", "ccorder"])
    P.cc(lambda e: e.collective_compute("AllGather", ALU.bypass, replica_groups=[list(range(NCORES))], ins=[vs[:, :]], outs=[vg[:, :]]),
         w=["vg", "ccorder"])
    qt = P.sb("qt", [128, W2], BF16)
    kt = P.sb("kt", [128, S], BF16)
    vt = P.sb("vt", [128, NKT, 130], BF16)
    ot = P.sb("ot", [128, 4, 128], BF16)
    otT = P.sb("otT", [128, 512], BF16)
    E = [P.sb("E%d" % i, [128, 512], BF16) for i in range(4)]
    dg = P.sb("dg", [128, 4, 512], F32)
    mc = P.sb("mc", [128, 16], F32)
    mt = P.sb("mt", [128, 8, 4, 512], BF16)
    idb = P.sb("idb", [128, 128], BF16)
    lam4 = P.sb("lam4", [128, 4, 64], F32)
    gs_bc = P.sb("gs_bc", [128, 128], F32)
    sm = P.sb("sm", [128, 16], F32)
    o0 = P.sb("o0", [128, 128], F32)
    od = P.sb("od", [128, 128], F32)
    sq = P.sb("sq", [128, 128], F32)
    o0s = P.sb("o0s", [128, 4, 128], F32)
    psS = [P.ps("psS%d" % i, [128, 512], F32) for i in range(3)]
    acc = [P.ps("acc%d" % i, [128, 512], F32) for i in range(4)]
    pT = P.ps("pT", [128, 512], BF16)

    P.dma("sp", dg[:].rearrange("p a q -> p (a q)"), diag[:, :], w=["dg"])
    P.dma("sp", mc[:], mcol[:, :], w=["mc"])
    for m in range(8):
        P.add("dve", lambda e, m=m: e.tensor_scalar(out=mt[:, m, :, :], in0=dg[:], scalar1=mc[:, 8 + m:9 + m], scalar2=mc[:, m:m + 1],
                                                    op0=ALU.mult, op1=ALU.add), r=["dg", "mc"], w=["consts"])
    P.dma("pool", idb[:], ident[:, :], w=["consts"])
    P.dma("sp", lam4[:].rearrange("p a d -> p (a d)"), lamv.rearrange("a d -> (a d)").partition_broadcast(128), w=["lam4"])
    P.dma("sp", gs_bc[:], gsub[0, :].partition_broadcast(128), w=["gs"])
    P.add("dve", lambda e: e.tensor_scalar(out=gs_bc[:], in0=gs_bc[:], scalar1=1.0 - lambda_init, scalar2=None, op0=ALU.mult),
          r=["gs"], w=["consts"])
    P.add("dve", lambda e: e.tensor_tensor(out=o0[:, 0:64], in0=lam4[:, 0, :], in1=lam4[:, 1, :], op=ALU.mult), r=["lam4"], w=["o0"])
    P.add("dve", lambda e: e.reduce_sum(out=sm[:, 0:1], in_=o0[:, 0:64], axis=AX.X), r=["o0"], w=["sm"])
    P.add("dve", lambda e: e.tensor_tensor(out=o0[:, 64:128], in0=lam4[:, 2, :], in1=lam4[:, 3, :], op=ALU.mult), r=["lam4"], w=["o0"])
    P.add("dve", lambda e: e.reduce_sum(out=sm[:, 1:2], in_=o0[:, 64:128], axis=AX.X), r=["o0"], w=["sm"])
    P.add("act", lambda e: e.activation(out=sm[:, 0:2], in_=sm[:, 0:2], func=AF.Exp), r=["sm"], w=["sm"])
    P.add("dve", lambda e: e.tensor_tensor(out=sm[:, 2:3], in0=sm[:, 1:2], in1=sm[:, 0:1], op=ALU.subtract), r=["sm"], w=["sm"])
    P.add("dve", lambda e: e.tensor_scalar(out=sm[:, 3:4], in0=sm[:, 2:3], scalar1=-lambda_init, scalar2=None, op0=ALU.add),
          r=["sm"], w=["consts2"])
    P.add("dve", lambda e: e.memset(vt[:, :, 128:129], 1.0), w=["vt1"])

    ei = 0
    si = 0
    for b in range(2):
        for h in range(16):
            P.dma("sp", qt[:], qs[h * 128:(h + 1) * 128, b * W2:(b + 1) * W2], w=["qt"])
            for m in range(8):
                P.dma("sp", kt[:, m * W2:(m + 1) * W2], kg[m * D + h * 128:m * D + (h + 1) * 128, b * W2:(b + 1) * W2], r=["kg"], w=["kt"])
                P.dma("sp", vt[:, m * KPM:(m + 1) * KPM, 0:128],
                      vg[m * NTOK + b * W2:m * NTOK + (b + 1) * W2, h * 128:(h + 1) * 128].rearrange("(n p) d -> p n d", p=128),
                      r=["vg", "vt1"], w=["vt"])
            for jj in range(NB2):
                tiles = [(jb, m, k4) for jb in range(jj + 1) for m in range(8) for k4 in range(4)]
                for c in range(2):
                    for idx, (jb, m, k4) in enumerate(tiles):
                        ki = m * KPM + jb * 4 + k4
                        pS = psS[si % 3]
                        pk = ("psS", si % 3)
                        si += 1
                        P.mm(pS[:, :], kt[c * 64:(c + 1) * 64, ki * 128:(ki + 1) * 128],
                             qt[c * 64:(c + 1) * 64, jj * 512:(jj + 1) * 512], True, True, r=["qt", "kt"], w=[pk])
                        Et = E[ei % 4]
                        ek = ("E", ei % 4)
                        ei += 1
                        P.add("act", lambda e, Et=Et, pS=pS: e.activation(out=Et[:], in_=pS[:, :], func=AF.Exp), r=[pk], w=[ek])
                        if jb == jj:
                            P.add("dve", lambda e, Et=Et, m=m, k4=k4: e.tensor_tensor(out=Et[:], in0=Et[:], in1=mt[:, m, k4, :], op=ALU.mult),
                                  r=[ek, "consts"], w=[ek])
                        for qi in range(4):
                            P.mm(acc[qi][:, 0:129], Et[:, qi * 128:(qi + 1) * 128], vt[:, ki, 0:129],
                                 idx == 0, idx == len(tiles) - 1, r=[ek, "vt", "vt1"], w=[("acc", qi)])
                    for qi in range(4):
                        a = acc[qi]
                        ak = ("acc", qi)
                        P.add("dve", lambda e, a=a: e.reciprocal(sm[:, 4:5], a[:, 128:129]), r=[ak], w=["sm"])
                        if c == 0:
                            P.add("act", lambda e, a=a, qi=qi: e.activation(out=o0s[:, qi, :], in_=a[:, 0:128], func=AF.Copy, scale=sm[:, 4:5]),
                                  r=[ak, "sm"], w=["o0s"])
                            continue
                        P.add("dve", lambda e: e.tensor_tensor(out=sm[:, 6:7], in0=sm[:, 4:5], in1=sm[:, 3:4], op=ALU.mult), r=["sm", "consts2"], w=["sm"])
                        P.add("dve", lambda e, a=a, qi=qi: e.scalar_tensor_tensor(out=od[:], in0=a[:, 0:128], scalar=sm[:, 6:7], in1=o0s[:, qi, :],
                                                                                  op0=ALU.mult, op1=ALU.add), r=[ak, "sm", "o0s"], w=["od"])
                        P.add("dve", lambda e: e.memset(sm[:, 7:8], 0.0), w=["sm"])
                        P.add("act", lambda e: e.activation(out=sq[:], in_=od[:], func=AF.Square, accum_out=sm[:, 7:8]), r=["od", "sm"], w=["sq", "sm"])
                        P.add("act", lambda e: e.activation(out=sm[:, 7:8], in_=sm[:, 7:8], func=AF.Sqrt, bias=EPS_AP[0][:, 0:1], scale=1.0 / 128),
                              r=["sm", "eps"], w=["sm"])
                        P.add("dve", lambda e: e.reciprocal(sm[:, 7:8], sm[:, 7:8]), r=["sm"], w=["sm"])
                        P.add("dve", lambda e, qi=qi: e.scalar_tensor_tensor(out=ot[:, qi, :], in0=od[:], scalar=sm[:, 7:8], in1=gs_bc[:],
                                                                             op0=ALU.mult, op1=ALU.mult), r=["od", "sm", "consts"], w=["ot"])
                for qi in range(4):
                    P.add("pe", lambda e, qi=qi: e.transpose(pT[:, qi * 128:(qi + 1) * 128], ot[:, qi, :], idb[:, :]), r=["ot", "consts"], w=["pT"])
                P.add("act", lambda e: e.activation(out=otT[:], in_=pT[:, :], func=AF.Copy), r=["pT"], w=["otT"])
                P.dma("sp", os_[h * 128:(h + 1) * 128, (b * NB2 + jj) * T:(b * NB2 + jj + 1) * T], otT[:], r=["otT"], w=["os"])
    P.emit()


def phase_c(sess, A, NTOK, NP):
    NFF = 11 * NP
    NT = NTOK // T
    nc = sess.nc
    xT, oT, cols, ident, w_r, w_o, w_gu, w_dn, outT = [A[k] for k in
        ("x2s", "os", "cols3", "ident", "w_r", "w_o", "w_gu3", "w_dn3", "outT")]
    P = Prog(sess, "c_")
    _mk_eps(P)
    C = Common(P, nc)
    xt, ht = C.xt, C.ht
    cst = P.sb("cst", [128, 32], F32)
    P.dma("sp", cst[:], cols[:, :], w=["consts"])
    G_MOE, G_FIN = 0, 16
    idf = P.sb("idf", [128, 128], F32)
    P.dma("sp", idf[:], ident[:, :], w=["consts"])
    wr = P.sb("wr", [128, 16, 8], BF16)
    P.dma("pool", wr[:].rearrange("p k e -> p (k e)"), w_r[:, :], w=["consts"])
    ob = P.sb("ob", [128, KC, T], BF16)
    scr = P.sb("scr", [128, max(NFF, 1), T], BF16)
    tmpg = P.sb("tmpg", [128, T], F32)
    tmpf = P.sb("tmpf", [128, T], F32)
    lg = P.sb("lg", [128, 8], F32)
    lg2 = P.sb("lg2", [128, 8], F32)
    sm = P.sb("sm", [128, 8], F32)
    gx = P.sb("gx", [128, 8, 128], F32)
    gbc = P.sb("gbc", [128, 8, T], F32)
    xT_v = xT.rearrange("(k p) t -> p k t", p=128)
    oT_v = oT.rearrange("(k p) t -> p k t", p=128)
    outT_v = outT.rearrange("(k p) t -> p k t", p=128)

    for it in range(NT):
        t0 = it * T
        P.dma("sp", xt[:], xT_v[:, :, t0:t0 + T], w=["xt"])
        P.dma("sp", ob[:], oT_v[:, :, t0:t0 + T], w=["ob"])
        for fo in range(16):
            wt, wk = C.wload(w_o[fo], 2048)
            bk, bkey = lhs_matmul_group(P, C, wt, wk, KC, lambda k: ob[:, k, :], ["ob"])
            P.add("dve", lambda e, fo=fo, bk=bk: e.tensor_tensor(out=xt[:, fo, :], in0=xt[:, fo, :], in1=bk[:, :], op=ALU.add),
                  r=["xt", bkey], w=["xt"])
        C.rmsnorm(cst[:, G_MOE:G_MOE + 16], "m")
        for s in range(4):
            bk, bkey = C.nb()
            for kc in range(KC):
                P.mm(bk[:, 0:8], ht[:, kc, s * 128:(s + 1) * 128], wr[:, kc, :], kc == 0, kc == KC - 1, r=["ht", "consts"], w=[bkey])
            P.add("dve", lambda e, bk=bk: e.tensor_copy(out=lg[:], in_=bk[:, 0:8]), r=[bkey], w=["lg"])
            P.add("dve", lambda e: e.reduce_max(out=sm[:, 0:1], in_=lg[:], axis=AX.X), r=["lg"], w=["sm"])
            P.add("dve", lambda e: e.tensor_scalar(out=lg2[:], in0=lg[:], scalar1=sm[:, 0:1], scalar2=1e9, op0=ALU.subtract, op1=ALU.mult),
                  r=["lg", "sm"], w=["lg2"])
            P.add("dve", lambda e: e.tensor_scalar(out=lg2[:], in0=lg2[:], scalar1=1.0, scalar2=0.0, op0=ALU.add, op1=ALU.max),
                  r=["lg2"], w=["lg2"])
            P.add("dve", lambda e: e.scalar_tensor_tensor(out=lg2[:], in0=lg2[:], scalar=-1e30, in1=lg[:], op0=ALU.mult, op1=ALU.add),
                  r=["lg2", "lg"], w=["lg2"])
            P.add("dve", lambda e: e.reduce_max(out=sm[:, 1:2], in_=lg2[:], axis=AX.X), r=["lg2"], w=["sm"])
            P.add("dve", lambda e: e.tensor_scalar(out=lg2[:], in0=lg[:], scalar1=sm[:, 1:2], scalar2=1e9, op0=ALU.subtract, op1=ALU.mult),
                  r=["lg", "sm"], w=["lg2"])
            P.add("dve", lambda e: e.tensor_scalar(out=lg2[:], in0=lg2[:], scalar1=1.0, scalar2=0.0, op0=ALU.add, op1=ALU.max),
                  r=["lg2"], w=["lg2"])
            P.add("dve", lambda e: e.tensor_scalar(out=lg2[:], in0=lg2[:], scalar1=1.0, scalar2=None, op0=ALU.min), r=["lg2"], w=["lg2"])
            P.add("dve", lambda e: e.tensor_scalar(out=sm[:, 2:3], in0=sm[:, 0:1], scalar1=-1.0, scalar2=None, op0=ALU.mult), r=["sm"], w=["sm"])
            P.add("act", lambda e: e.activation(out=lg[:], in_=lg[:], func=AF.Exp, bias=sm[:, 2:3], scale=1.0), r=["lg", "sm"], w=["lg"])
            P.add("dve", lambda e: e.tensor_tensor(out=lg[:], in0=lg[:], in1=lg2[:], op=ALU.mult), r=["lg", "lg2"], w=["lg"])
            P.add("dve", lambda e: e.reduce_sum(out=sm[:, 3:4], in_=lg[:], axis=AX.X), r=["lg"], w=["sm"])
            P.add("dve", lambda e: e.reciprocal(sm[:, 3:4], sm[:, 3:4]), r=["sm"], w=["sm"])
            P.add("dve", lambda e: e.tensor_scalar(out=lg[:], in0=lg[:], scalar1=sm[:, 3:4], scalar2=None, op0=ALU.mult), r=["lg", "sm"], w=["lg"])
            P.add("dve", lambda e: e.tensor_copy(out=gx[:], in_=lg[:].unsqueeze(2).to_broadcast([128, 8, 128])), r=["lg"], w=["gx"])
            for eh in range(2):
                b2, b2k = C.nb()
                for ee in range(4):
                    P.mm(b2[:, ee * 128:(ee + 1) * 128], gx[:, eh * 4 + ee, :], idf[:, :], True, True, r=["gx", "consts"], w=[b2k])
                P.add("act", lambda e, b2=b2, eh=eh, s=s: e.activation(out=gbc[:, eh * 4:eh * 4 + 4, s * 128:(s + 1) * 128],
                                                                      in_=b2[:, :].rearrange("p (g t) -> p g t", t=128), func=AF.Copy),
                      r=[b2k], w=["gbc"])
        for ex in range(8):
            for j in range(NFF):
                wt, wk = C.wload(w_gu[(ex * NFF + j) * 2], 2048)
                bg, bgk = lhs_matmul_group(P, C, wt, wk, KC, lambda k: ht[:, k, :], ["ht"])
                wt, wk = C.wload(w_gu[(ex * NFF + j) * 2 + 1], 2048)
                bu, buk = lhs_matmul_group(P, C, wt, wk, KC, lambda k: ht[:, k, :], ["ht"])
                P.add("act", lambda e, bg=bg: e.activation(out=tmpg[:], in_=bg[:, :], func=AF.Silu), r=[bgk], w=["tmpg"])
                P.add("dve", lambda e, bu=bu, j=j: e.tensor_tensor(out=scr[:, j, :], in0=tmpg[:], in1=bu[:, :], op=ALU.mult),
                      r=["tmpg", buk], w=[("scr", j)])
            for fo in range(16):
                bk, bkey = C.nb()
                for pc in range(NP):
                    wt, wk = C.wload(w_dn[(ex * 16 + fo) * NP + pc], 1408)
                    lhs_matmul_group(P, C, wt, wk, 11, lambda k: scr[:, k, :], [("scr", x) for x in range(pc * 11, pc * 11 + 11)],
                                     bk=bk, bkey=bkey, first=(pc == 0), last=(pc == NP - 1), kbase=pc * 11)
                P.add("dve", lambda e, bk=bk, ex=ex: e.tensor_tensor(out=tmpf[:], in0=bk[:, :], in1=gbc[:, ex, :], op=ALU.mult),
                      r=[bkey, "gbc"], w=["tmpf"])
                P.add("dve", lambda e, fo=fo: e.tensor_tensor(out=xt[:, fo, :], in0=xt[:, fo, :], in1=tmpf[:], op=ALU.add),
                      r=["xt", "tmpf"], w=["xt"])
        C.rmsnorm(cst[:, G_FIN:G_FIN + 16], "z")
        for kc in range(KC):
            P.add("dve", lambda e, kc=kc: e.scalar_tensor_tensor(out=xt[:, kc, :], in0=xt[:, kc, :], scalar=cst[:, G_FIN + kc:G_FIN + kc + 1],
                                                                   in1=C.rstd[:], op0=ALU.mult, op1=ALU.mult), r=["xt", "rstd", "consts"], w=["xt"])
        P.dma("sp", outT_v[:, :, t0:t0 + T], xt[:], r=["xt"])
    P.emit()


def build_fused(NTOK, NP, S):
    NFF = 11 * NP
    NB = NTOK // T
    nc = bass.Bass("TRN2", target_bir_lowering=False)
    A = {}

    def inp(name, shape, dt=F32):
        A[name] = nc.dram_tensor(name, list(shape), dt, kind="ExternalInput").ap()

    inp("xT", [D, NTOK]); inp("xh", [NB, D, 16]); inp("pos", [1, NTOK], I32); inp("invcnt", [1, NB * 64])
    inp("cols", [128, 64]); inp("lng", [1, 1024]); inp("lnb", [1, 1024]); inp("bsp", [1, 1024])
    inp("wsT", [128, 1024]); inp("tri", [128, 128]); inp("wpool", [128, 2048]); inp("rotP", [128, 128])
    inp("w_inA", [16, 128, 2048]); inp("w_inV", [8, 128, 2048]); inp("w_out", [16, 128, 2048])
    inp("w_gu", [2 * NFF, 128, 2048]); inp("w_dn", [16 * NP, 128, 1408]); inp("w_qk", [32, 128, 2048]); inp("w_v", [16, 128, 2048])
    inp("lamv", [4, 64]); inp("gsub", [1, 128]); inp("diag", [128, 2048]); inp("mcol", [128, 16]); inp("ident", [128, 128])
    inp("cols3", [128, 32]); inp("w_r", [128, 128]); inp("w_o", [16, 128, 2048])
    inp("w_gu3", [8 * 2 * NFF, 128, 2048]); inp("w_dn3", [8 * 16 * NP, 128, 1408])
    A["outT"] = nc.dram_tensor("outT", [D, NTOK], F32, kind="ExternalOutput").ap()
    A["x2s"] = nc.dram_tensor("x2s", [D, NTOK], F32).ap()
    A["qs"] = nc.dram_tensor("qs", [D, NTOK], BF16).ap()
    A["ks"] = nc.dram_tensor("ks", [D, NTOK], BF16).ap()
    A["vs"] = nc.dram_tensor("vs", [NTOK, D], BF16).ap()
    A["os"] = nc.dram_tensor("os", [D, NTOK], BF16).ap()
    A["kg"] = nc.dram_tensor("kg", [NCORES * D, NTOK], BF16, addr_space="Shared").ap()
    A["vg"] = nc.dram_tensor("vg", [NCORES * NTOK, D], BF16, addr_space="Shared").ap()
    lambda_init = 0.8 - 0.6 * math.exp(-0.3 * 1)
    sess = Sess(nc)
    phase_a(sess, A, NTOK, NP)
    phase_b(sess, A, NTOK, S, lambda_init)
    phase_c(sess, A, NTOK, NP)
    sess.close()
    return nc


def _lhs_chunks(W):
    K, N = W.shape
    return np.ascontiguousarray(W.reshape(K // 128, 128, N // 128, 128).transpose(2, 1, 0, 3)).reshape(N // 128, 128, (K // 128) * 128)


def _rhs_pieces(W):
    K, N = W.shape
    a = W.reshape(4, 4, 128, N // 512, 512).transpose(3, 0, 2, 1, 4)
    return np.ascontiguousarray(a).reshape((N // 512) * 4, 128, 2048)


def _dn_pieces(W, NP):
    a = W.reshape(NP, 11, 128, 16, 128).transpose(3, 0, 2, 1, 4)
    return np.ascontiguousarray(a).reshape(16 * NP, 128, 1408)


def _colpack(v):
    return np.ascontiguousarray(np.asarray(v, np.float32).reshape(-1, 128).T)


def kernel(x, positions,
           ev_norm_mix, ev_w_in, ev_w_pool, ev_pool_scale, ev_ln_g, ev_ln_b,
           ev_w_spatial, ev_b_spatial, ev_w_out, ev_norm_ffn, ev_w_gate, ev_w_up, ev_w_down,
           od_norm_attn, od_w_qkv, od_lam_q1, od_lam_k1, od_lam_q2, od_lam_k2, od_subln_g,
           od_w_o, od_norm_moe, od_w_router, od_we_gate, od_we_up, od_we_down,
           final_norm):
    f32 = np.float32
    x = np.asarray(x, f32)
    B, S, _ = x.shape
    assert B == 2
    DFF = np.asarray(ev_w_gate).shape[-1]
    NP = DFF // 1408
    NFF = 11 * NP
    NTOK = (B * S) // NCORES
    NB2 = NTOK // T // 2
    positions = np.asarray(positions, np.int32)
    A = lambda a: np.asarray(a, f32)

    w_in = A(ev_w_in)[0]
    cols = np.zeros((128, 64), f32)
    cols[:, 0:16] = _colpack(A(ev_norm_mix)[0])
    cols[:, 16:24] = _colpack(A(ev_pool_scale)[0])
    cols[:, 24:40] = _colpack(A(ev_norm_ffn)[0])
    cols[:, 40:56] = _colpack(A(od_norm_attn)[0])
    inv_freq = (10000.0 ** (-np.arange(0, 64, 2, dtype=f32) / 64)).astype(f32)
    cols[:, 56] = np.tile(inv_freq, 4)
    cols[:, 57] = np.tile(np.concatenate([-np.ones(32, f32), np.ones(32, f32)]), 2)
    tri = np.triu(np.ones((128, 128), f32))
    rotP = np.zeros((128, 128), f32)
    for m in range(128):
        k = (m // 64) * 64 + ((m % 64) + 32) % 64
        rotP[k, m] = 1.0
    wsT = np.ascontiguousarray(A(ev_w_spatial)[0].transpose(2, 0, 1)).reshape(128, 1024)
    wp = A(ev_w_pool)[0].reshape(4, 2, 128, 2, 128).transpose(2, 0, 1, 3, 4)
    wpool = np.ascontiguousarray(wp).reshape(128, 2048)
    wqkv = A(od_w_qkv)[0]
    wgu = np.empty((2 * NFF, 128, 2048), f32)
    wgu[0::2] = _lhs_chunks(A(ev_w_gate)[0])
    wgu[1::2] = _lhs_chunks(A(ev_w_up)[0])
    kk = np.arange(128)[:, None, None] + 128 * np.arange(4)[None, :, None]
    diag = (kk <= np.arange(512)[None, None, :]).astype(f32).reshape(128, 2048)
    cols3 = np.zeros((128, 32), f32)
    cols3[:, 0:16] = _colpack(A(od_norm_moe)[0])
    cols3[:, 16:32] = _colpack(A(final_norm))
    w_r = np.ascontiguousarray(A(od_w_router)[0].reshape(16, 128, 8).transpose(1, 0, 2)).reshape(128, 128)
    wgu3 = np.empty((8 * NFF * 2, 128, 2048), f32)
    wdn3 = np.empty((8 * 16 * NP, 128, 1408), f32)
    for e in range(8):
        wgu3[e * 2 * NFF:(e + 1) * 2 * NFF:2] = _lhs_chunks(A(od_we_gate)[0, e])
        wgu3[e * 2 * NFF + 1:(e + 1) * 2 * NFF:2] = _lhs_chunks(A(od_we_up)[0, e])
        wdn3[e * 16 * NP:(e + 1) * 16 * NP] = _dn_pieces(A(od_we_down)[0, e], NP)
    lamv = np.stack([A(od_lam_q1)[0], A(od_lam_k1)[0], A(od_lam_q2)[0], A(od_lam_k2)[0]], 0)
    shared = dict(
        cols=cols, lng=A(ev_ln_g)[0][None], lnb=A(ev_ln_b)[0][None], bsp=A(ev_b_spatial)[0].reshape(1, 1024),
        wsT=wsT, tri=tri, wpool=wpool, rotP=rotP,
        w_inA=np.concatenate([_lhs_chunks(w_in[:, 0:1024]), _lhs_chunks(w_in[:, 1024:2048])], 0),
        w_inV=_rhs_pieces(w_in[:, 2048:3072]), w_out=_lhs_chunks(A(ev_w_out)[0]), w_gu=wgu,
        w_dn=_dn_pieces(A(ev_w_down)[0], NP), w_qk=_lhs_chunks(wqkv[:, 0:4096]), w_v=_rhs_pieces(wqkv[:, 4096:6144]),
        lamv=lamv, gsub=A(od_subln_g)[0][None], diag=diag, ident=np.eye(128, dtype=f32),
        cols3=cols3, w_r=w_r, w_o=_lhs_chunks(A(od_w_o)[0]), w_gu3=wgu3, w_dn3=wdn3)
    in_maps = []
    for c in range(NCORES):
        xs, xhs, ps, ics = [], [], [], []
        for b in range(B):
            for jj in range(NB2):
                t0 = (8 * jj + c) * T
                xs.append(x[b, t0:t0 + T])
                xhs.append(x[b, t0 - 16:t0].T if t0 > 0 else np.zeros((D, 16), f32))
                ps.append(positions[b, t0:t0 + T])
                ic = np.zeros((4, 16), f32)
                for g, w in enumerate((2, 4, 8, 16)):
                    ic[g] = 1.0 / np.minimum(np.arange(16) + 1, w) if t0 == 0 else 1.0 / w
                ics.append(ic.reshape(64))
        mcol = np.zeros((128, 16), f32)
        mcol[:, 0:8] = (np.arange(8) < c).astype(f32)[None]
        mcol[:, 8 + c] = 1.0
        m = dict(shared)
        m.update(xT=np.ascontiguousarray(np.concatenate(xs, 0).T), xh=np.ascontiguousarray(np.stack(xhs, 0)),
                 pos=np.concatenate(ps)[None].copy(), invcnt=np.concatenate(ics)[None].copy(), mcol=mcol)
        in_maps.append(m)
    res = run_bass_kernel_spmd(build_fused(NTOK, NP, S), in_maps, core_ids=list(range(NCORES))).results
    out = np.empty((B, S, D), f32)
    for c in range(NCORES):
        o = res[c]["outT"].T
        i = 0
        for b in range(B):
            for jj in range(NB2):
                t0 = (8 * jj + c) * T
                out[b, t0:t0 + T] = o[i * T:(i + 1) * T]
                i += 1
    return out
```

```python
import contextlib
import math
import numpy as np
import ml_dtypes
import concourse.bass as bass
import concourse.mybir as mybir
from concourse.bass_utils import run_bass_kernel_spmd

F32, BF16, I32 = mybir.dt.float32, mybir.dt.bfloat16, mybir.dt.int32
AF = mybir.ActivationFunctionType
ALU = mybir.AluOpType
AX = mybir.AxisListType

D = 2048
KC = 16
T = 512
NCORES = 8
EPS = 1e-5
PI = math.pi


class Sess:
    ENG = {"pe": "tensor", "act": "scalar", "dve": "vector", "pool": "gpsimd", "sp": "sync"}

    def __init__(self, nc, ndma=10):
        self.nc, self.ndma = nc, ndma
        self.stack = contextlib.ExitStack()
        en = self.stack.enter_context
        self.sem = {k: en(nc.semaphore("p_" + k)) for k in ("pe", "act", "dve", "pool")}
        self.dsem = {k: [en(nc.semaphore("d_%s%d" % (k, q))) for q in range(ndma)] for k in ("sp", "pool")}
        self.ccsem = en(nc.semaphore("ccs"))
        self.cccnt = 0
        self.dtot = {}
        self.dnext = {"sp": 0, "pool": 0}
        self.cnt = {"pe": 0, "act": 0, "dve": 0, "pool": 0}
        self.waited = {k: {} for k in self.ENG}
        self.engobj = {k: getattr(nc, v) for k, v in self.ENG.items()}

    def close(self):
        self.stack.close()


class Prog:
    def __init__(self, sess, prefix):
        self.S = sess
        self.nc = sess.nc
        self.prefix = prefix
        self.ops = []
        self.stack = contextlib.ExitStack()

    def sb(self, name, shape, dt):
        return self.stack.enter_context(self.nc.sbuf_tensor(self.prefix + name, list(shape), dt))

    def ps(self, name, shape, dt=F32):
        return self.stack.enter_context(self.nc.psum_tensor(self.prefix + name, list(shape), dt))

    def add(self, eng, fn, r=(), w=(), dma=False):
        self.ops.append([eng, fn, tuple(r), tuple(w), dma])

    def dma(self, eng, out, in_, r=(), w=()):
        self.add(eng, lambda e: e.dma_start(out=out, in_=in_), r, w, dma=True)

    def cc(self, fn, r=(), w=()):
        self.add("pool", fn, r, w, dma="cc")

    def mm(self, out, lhsT, rhs, start, stop, r=(), w=()):
        self.add("pe", lambda e: e.matmul(out, lhsT, rhs, start=start, stop=stop), r, w)

    def emit(self):
        S, ops = self.S, self.ops
        n = len(ops)
        last_w, readers = {}, {}
        deps = [None] * n
        for i, (eng, fn, r, w, dma) in enumerate(ops):
            d = set()
            for b in r:
                if b in last_w:
                    d.add(last_w[b])
            for b in w:
                if b in last_w:
                    d.add(last_w[b])
                d.update(readers.get(b, ()))
            d.discard(i)
            if eng == "pe":
                d = {j for j in d if not (ops[j][0] == "pe" and not ops[j][4])}
            deps[i] = d
            for b in r:
                readers.setdefault(b, []).append(i)
            for b in w:
                last_w[b] = i
                readers[b] = []
        marked = [False] * n
        for i in range(n):
            for j in deps[i]:
                marked[j] = True
        for k in ("pe", "act", "dve", "pool"):
            for i in range(n - 1, -1, -1):
                if ops[i][0] == k and not ops[i][4]:
                    marked[i] = True
                    break
        token = [None] * n

        def do_wait(eng, s, v):
            if S.waited[eng].get(s.name, 0) >= v:
                return
            S.engobj[eng].wait_ge(s, v)
            S.waited[eng][s.name] = v

        for i, (eng, fn, r, w, dma) in enumerate(ops):
            for j in sorted(deps[i]):
                s, v = token[j]
                do_wait(eng, s, v)
            if dma == "cc":
                ins = fn(S.engobj[eng])
                ins.then_inc(S.ccsem)
                S.cccnt += 1
                token[i] = (S.ccsem, S.cccnt)
            elif dma:
                q = S.dnext[eng]
                S.dnext[eng] = (q + 1) % S.ndma
                s = S.dsem[eng][q]
                prev = S.dtot.get(s.name, 0)
                if prev:
                    do_wait(eng, s, prev)
                ins = fn(S.engobj[eng])
                ins.then_inc(s, 16)
                S.dtot[s.name] = prev + 16
                token[i] = (s, prev + 16)
            else:
                ins = fn(S.engobj[eng])
                if marked[i]:
                    S.cnt[eng] += 1
                    ins.then_inc(S.sem[eng], 1)
                    token[i] = (S.sem[eng], S.cnt[eng])
        for eng in S.ENG:
            for k in S.sem:
                if S.cnt[k]:
                    do_wait(eng, S.sem[k], S.cnt[k])
            for k in S.dsem:
                for s in S.dsem[k]:
                    if S.dtot.get(s.name, 0):
                        do_wait(eng, s, S.dtot[s.name])
            if S.cccnt:
                do_wait(eng, S.ccsem, S.cccnt)
        self.stack.close()


def _bc(ap, shape):
    return ap.to_broadcast(list(shape))


class Common:
    def __init__(self, P, nc, nw=4):
        self.P, self.nc = P, nc
        self.nw = nw
        self.wr = [P.sb("wring%d" % i, [128, 2048], BF16) for i in range(nw)]
        self.wi = 0
        self.bank = [P.ps("bank%d" % i, [128, 512], F32) for i in range(8)]
        self.bi = 0
        self.xt = P.sb("xt", [128, KC, T], F32)
        self.ht = P.sb("ht", [128, KC, T], BF16)
        self.rstd = P.sb("rstd", [128, T], F32)
        self.ones = P.sb("ones", [128, 128], BF16)
        P.add("dve", lambda e: e.memset(self.ones[:], 1.0), w=["ones"])

    def wload(self, src_ap, nelem):
        i = self.wi
        self.wi = (i + 1) % self.nw
        t = self.wr[i]
        self.P.dma("pool", t[:, 0:nelem], src_ap, w=[("w", i)])
        return t, ("w", i)

    def nb(self):
        i = self.bi
        self.bi = (i + 1) % 8
        return self.bank[i], ("ps", i)

    def rmsnorm(self, gcol, tag):
        P, xt, ht = self.P, self.xt, self.ht
        P.add("act", lambda e: e.activation(out=ht[:], in_=xt[:], func=AF.Square), r=["xt"], w=["ht"])
        bk, bkey = self.nb()
        for kc in range(KC):
            P.mm(bk[:, :], self.ones[:, :], ht[:, kc, :], kc == 0, kc == KC - 1, r=["ht", "ones"], w=[bkey])
        rs = self.rstd
        P.add("act", lambda e: e.activation(out=rs[:], in_=bk[:, :], func=AF.Sqrt, bias=EPS_AP[0][:, 0:1], scale=1.0 / D),
              r=[bkey, "eps"], w=["rstd"])
        P.add("dve", lambda e: e.reciprocal(rs[:], rs[:]), r=["rstd"], w=["rstd"])
        for kc in range(KC):
            P.add("dve", lambda e, kc=kc: e.scalar_tensor_tensor(out=ht[:, kc, :], in0=xt[:, kc, :], scalar=gcol[:, kc:kc + 1],
                                                                   in1=rs[:], op0=ALU.mult, op1=ALU.mult),
                  r=["xt", "rstd", "consts"], w=["ht"])


EPS_AP = [None]


def _mk_eps(P):
    t = P.sb("epsc", [128, 1], F32)
    P.add("dve", lambda e: e.memset(t[:], EPS), w=["eps"])
    EPS_AP[0] = t


def lhs_matmul_group(P, C, wt, wkey, nk, rhs_fn, rkeys, bk=None, bkey=None, first=True, last=True, kbase=0, ncols=T):
    if bk is None:
        bk, bkey = C.nb()
    wv = wt[:, 0:nk * 128].rearrange("p (k m) -> p k m", m=128)
    for k in range(nk):
        P.mm(bk[:, 0:ncols], wv[:, k, :], rhs_fn(kbase + k), first and k == 0, last and k == nk - 1,
             r=[wkey] + list(rkeys), w=[bkey])
    return bk, bkey


def phase_a(sess, A, NTOK, NP):
    NFF = 11 * NP
    NT = NTOK // T
    nc = sess.nc
    NB = NTOK // T
    xT, xh, pos, invcnt, cols, lng, lnb, bsp, wsT, tri, wpool, rotP = [A[k] for k in
        ("xT", "xh", "pos", "invcnt", "cols", "lng", "lnb", "bsp", "wsT", "tri", "wpool", "rotP")]
    w_inA, w_inV, w_out, w_gu, w_dn, w_qk, w_v = [A[k] for k in ("w_inA", "w_inV", "w_out", "w_gu", "w_dn", "w_qk", "w_v")]
    x2T, qT, kT, vtok = A["x2s"], A["qs"], A["ks"], A["vs"]
    P = Prog(sess, "a_")
    _mk_eps(P)
    C = Common(P, nc)
    xt, ht = C.xt, C.ht
    cst = P.sb("cst", [128, 64], F32)
    P.dma("sp", cst[:], cols[:, :], w=["consts"])
    G_MIX, PSC, G_FFN, G_ATT, IFQ, SGN = 0, 16, 24, 40, 56, 57
    lng_bc = P.sb("lng_bc", [128, 1024], F32)
    lnb_bc = P.sb("lnb_bc", [128, 1024], F32)
    bsp_bc = P.sb("bsp_bc", [128, 8, 128], F32)
    icn_bc = P.sb("icn_bc", [128, NB, 4, 16], F32)
    P.dma("sp", lng_bc[:], lng[0, :].partition_broadcast(128), w=["consts"])
    P.dma("sp", lnb_bc[:], lnb[0, :].partition_broadcast(128), w=["consts"])
    P.dma("sp", bsp_bc[:].rearrange("p g t -> p (g t)"), bsp[0, :].partition_broadcast(128), w=["consts"])
    P.dma("sp", icn_bc[:].rearrange("p b g t -> p (b g t)"), invcnt[0, :].partition_broadcast(128), w=["consts"])
    wsf = P.sb("wsf", [128, 8, 128], F32)
    trif = P.sb("trif", [128, 128], F32)
    wsm = P.sb("wsm", [128, 8, 128], BF16)
    P.dma("sp", wsf[:].rearrange("p g t -> p (g t)"), wsT[:, :], w=["wsf"])
    P.dma("sp", trif[:], tri[:, :], w=["trif"])
    P.add("dve", lambda e: e.tensor_tensor(out=wsm[:], in0=wsf[:], in1=trif[:].unsqueeze(1).to_broadcast([128, 8, 128]), op=ALU.mult),
          r=["wsf", "trif"], w=["consts"])
    wpl = P.sb("wpl", [128, 16, 128], BF16)
    P.dma("pool", wpl[:].rearrange("p a m -> p (a m)"), wpool[:, :], w=["consts"])
    rot = P.sb("rot", [128, 128], BF16)
    P.dma("pool", rot[:], rotP[:, :], w=["consts"])

    scr = P.sb("scr", [128, max(44, 32), T], BF16)
    PO, YP, YG, UU = 0, 8, 16, 24
    zp = P.sb("zp", [128, 8, 16 + T], F32)
    pa = P.sb("pa", [128, 16 + T], F32)
    pb = P.sb("pb", [128, 16 + T], F32)
    vg = P.sb("vg", [128, 1024], F32)
    vln = P.sb("vln", [128, 1024], BF16)
    st = P.sb("st", [128, 8], F32)
    tmpf = P.sb("tmpf", [128, T], F32)
    tmpg = P.sb("tmpg", [128, T], F32)
    posi = P.sb("posi", [128, T], I32)
    ang = P.sb("ang", [128, T], F32)
    cosT = P.sb("cosT", [128, T], F32)
    sinT = P.sb("sinT", [128, T], F32)
    qsb = P.sb("qsb", [128, T], BF16)
    qo = [P.sb("qo%d" % i, [128, T], BF16) for i in range(2)]
    vsb = P.sb("vsb", [128, 4, D], BF16)
    negpi = P.sb("negpi", [128, 1], F32)
    P.add("dve", lambda e: e.memset(negpi[:], -PI), w=["consts"])

    xT_v = xT.rearrange("(k p) t -> p k t", p=128)
    x2T_v = x2T.rearrange("(k p) t -> p k t", p=128)

    xhs = P.sb("xhs", [128, KC, 16], F32)
    hh = P.sb("hh", [128, KC, 16], BF16)
    rsh = P.sb("rsh", [128, 16], F32)
    xh_v = xh.rearrange("b (k p) t -> b p k t", p=128)

    for it in range(NT):
        t0 = it * T
        P.dma("sp", xt[:], xT_v[:, :, t0:t0 + T], w=["xt"])
        C.rmsnorm(cst[:, G_MIX:G_MIX + 16], "m")
        P.dma("sp", xhs[:], xh_v[it], w=["xhs"])
        P.add("act", lambda e: e.activation(out=hh[:], in_=xhs[:], func=AF.Square), r=["xhs"], w=["hh"])
        bk, bkey = C.nb()
        for kc in range(KC):
            P.mm(bk[:, 0:16], C.ones[:, :], hh[:, kc, :], kc == 0, kc == KC - 1, r=["hh", "ones"], w=[bkey])
        P.add("act", lambda e, bk=bk: e.activation(out=rsh[:], in_=bk[:, 0:16], func=AF.Sqrt, bias=EPS_AP[0][:, 0:1], scale=1.0 / D),
              r=[bkey, "eps"], w=["rsh"])
        P.add("dve", lambda e: e.reciprocal(rsh[:], rsh[:]), r=["rsh"], w=["rsh"])
        for kc in range(KC):
            P.add("dve", lambda e, kc=kc: e.scalar_tensor_tensor(out=hh[:, kc, :], in0=xhs[:, kc, :], scalar=cst[:, G_MIX + kc:G_MIX + kc + 1],
                                                                   in1=rsh[:], op0=ALU.mult, op1=ALU.mult), r=["xhs", "rsh", "consts"], w=["hh"])
        for j in range(8):
            wt, wk = C.wload(w_inA[j], 2048)
            bk, bkey = lhs_matmul_group(P, C, wt, wk, KC, lambda k: ht[:, k, :], ["ht"])
            P.add("act", lambda e, j=j, bk=bk: e.activation(out=zp[:, j, 16:16 + T], in_=bk[:, :], func=AF.Copy), r=[bkey], w=["zp"])
            bk, bkey = lhs_matmul_group(P, C, wt, wk, KC, lambda k: hh[:, k, :], ["hh"], ncols=16)
            P.add("act", lambda e, j=j, bk=bk: e.activation(out=zp[:, j, 0:16], in_=bk[:, 0:16], func=AF.Copy), r=[bkey], w=["zp"])
        for j in range(8):
            wt, wk = C.wload(w_inA[8 + j], 2048)
            bk, bkey = lhs_matmul_group(P, C, wt, wk, KC, lambda k: ht[:, k, :], ["ht"])
            P.add("act", lambda e, j=j, bk=bk: e.activation(out=scr[:, UU + j, :], in_=bk[:, :], func=AF.Gelu),
                  r=[bkey], w=[("scr", UU + j)])
        for j in range(8):
            g = j // 2
            w = 2 << g
            z = zp[:, j, :]
            src, bufs, sh = z, [pa, pb], 1
            for stp in range(g + 1):
                dst = bufs[stp % 2]
                s_ = src
                P.add("dve", lambda e, dst=dst, s_=s_, sh=sh: e.tensor_tensor(out=dst[:, sh:16 + T], in0=s_[:, sh:16 + T],
                                                                               in1=s_[:, 0:16 + T - sh], op=ALU.add),
                      r=["zp", "pa", "pb"], w=["pa", "pb"])
                src = dst[:, :]
                sh *= 2
            P.add("dve", lambda e, j=j, src=src, w=w: e.scalar_tensor_tensor(out=scr[:, PO + j, :], in0=src[:, 16:16 + T], scalar=1.0 / w,
                                                                             in1=zp[:, j, 16:16 + T], op0=ALU.mult, op1=ALU.subtract),
                  r=["zp", "pa", "pb"], w=[("scr", PO + j)])
            if True:
                P.add("dve", lambda e, src=src, g=g, it=it: e.tensor_tensor(out=tmpf[:, 0:16], in0=src[:, 16:32], in1=icn_bc[:, it, g, :], op=ALU.mult),
                      r=["pa", "pb", "consts"], w=["tmpf"])
                P.add("dve", lambda e, j=j: e.tensor_tensor(out=scr[:, PO + j, 0:16], in0=tmpf[:, 0:16], in1=zp[:, j, 16:32], op=ALU.subtract),
                      r=["tmpf", "zp"], w=[("scr", PO + j)])
        for g in range(4):
            for do in range(2):
                bk, bkey = C.nb()
                for ci in range(2):
                    P.mm(bk[:, :], wpl[:, (g * 2 + ci) * 2 + do, :], scr[:, PO + 2 * g + ci, :], ci == 0, ci == 1,
                         r=["consts", ("scr", PO + 2 * g + ci)], w=[bkey])
                P.add("act", lambda e, bk=bk, g=g, do=do: e.activation(out=scr[:, YP + 2 * g + do, :], in_=bk[:, :], func=AF.Copy,
                                                                        scale=cst[:, PSC + 2 * g + do:PSC + 2 * g + do + 1]),
                      r=[bkey, "consts"], w=[("scr", YP + 2 * g + do)])
        for s in range(4):
            for n in range(2):
                bks = None
                bk, bkey = C.nb()
                for q in range(4):
                    wt, wk = C.wload(w_inV[n * 4 + q], 2048)
                    wv = wt[:, :].rearrange("p (k m) -> p k m", m=512)
                    for kk in range(4):
                        kc = 4 * q + kk
                        P.mm(bk[:, :], ht[:, kc, s * 128:(s + 1) * 128], wv[:, kk, :], kc == 0, kc == KC - 1,
                             r=["ht", wk], w=[bkey])
                P.add("act", lambda e, bk=bk, n=n: e.activation(out=vg[:, n * 512:(n + 1) * 512], in_=bk[:, :], func=AF.Gelu),
                      r=[bkey], w=["vg"])
            P.add("dve", lambda e: e.memset(st[:], 0.0), w=["st"])
            P.add("dve", lambda e: e.reduce_sum(out=st[:, 0:1], in_=vg[:], axis=AX.X), r=["vg"], w=["st"])
            P.add("act", lambda e: e.activation(out=tmpg[:, :], in_=vg[:, 0:512], func=AF.Square, accum_out=st[:, 1:2]),
                  r=["vg"], w=["tmpg", "st"])
            P.add("act", lambda e: e.activation(out=tmpg[:, :], in_=vg[:, 512:1024], func=AF.Square, accum_out=st[:, 2:3]),
                  r=["vg"], w=["tmpg", "st"])
            P.add("dve", lambda e: e.tensor_scalar(out=st[:, 3:4], in0=st[:, 0:1], scalar1=1.0 / 1024, scalar2=None, op0=ALU.mult),
                  r=["st"], w=["st"])
            P.add("dve", lambda e: e.tensor_tensor(out=st[:, 4:5], in0=st[:, 1:2], in1=st[:, 2:3], op=ALU.add), r=["st"], w=["st"])
            P.add("dve", lambda e: e.tensor_tensor(out=st[:, 5:6], in0=st[:, 3:4], in1=st[:, 3:4], op=ALU.mult), r=["st"], w=["st"])
            P.add("dve", lambda e: e.scalar_tensor_tensor(out=st[:, 6:7], in0=st[:, 4:5], scalar=1.0 / 1024, in1=st[:, 5:6],
                                                          op0=ALU.mult, op1=ALU.subtract), r=["st"], w=["st"])
            P.add("act", lambda e: e.activation(out=st[:, 6:7], in_=st[:, 6:7], func=AF.Sqrt, bias=EPS_AP[0][:, 0:1], scale=1.0),
                  r=["st", "eps"], w=["st"])
            P.add("dve", lambda e: e.reciprocal(st[:, 6:7], st[:, 6:7]), r=["st"], w=["st"])
            P.add("dve", lambda e: e.scalar_tensor_tensor(out=st[:, 7:8], in0=st[:, 3:4], scalar=-1.0, in1=st[:, 6:7],
                                                          op0=ALU.mult, op1=ALU.mult), r=["st"], w=["st"])
            P.add("act", lambda e: e.activation(out=vg[:], in_=vg[:], func=AF.Identity, bias=st[:, 7:8], scale=st[:, 6:7]),
                  r=["vg", "st"], w=["vg"])
            P.add("dve", lambda e: e.tensor_tensor(out=vg[:], in0=vg[:], in1=lng_bc[:], op=ALU.mult), r=["vg", "consts"], w=["vg"])
            P.add("dve", lambda e: e.tensor_tensor(out=vln[:], in0=vg[:], in1=lnb_bc[:], op=ALU.add), r=["vg", "consts"], w=["vln"])
            for gh in range(2):
                bk, bkey = C.nb()
                for gg in range(4):
                    g = gh * 4 + gg
                    P.mm(bk[:, gg * 128:(gg + 1) * 128], vln[:, g * 128:(g + 1) * 128], wsm[:, g, :], True, True,
                         r=["vln", "consts"], w=[bkey])
                bkv = bk[:, :].rearrange("p (g t) -> p g t", t=128)
                tv = tmpf[:, :].rearrange("p (g t) -> p g t", t=128)
                P.add("dve", lambda e, bkv=bkv, tv=tv, gh=gh: e.tensor_tensor(out=tv, in0=bkv, in1=bsp_bc[:, gh * 4:gh * 4 + 4, :], op=ALU.add),
                      r=[bkey, "consts"], w=["tmpf"])
                P.add("dve", lambda e, tv=tv, gh=gh, s=s: e.tensor_tensor(out=scr[:, YG + gh * 4:YG + gh * 4 + 4, s * 128:(s + 1) * 128], in0=tv,
                                                                           in1=scr[:, UU + gh * 4:UU + gh * 4 + 4, s * 128:(s + 1) * 128], op=ALU.mult),
                      r=["tmpf"] + [("scr", UU + gh * 4 + x) for x in range(4)], w=[("scr", YG + gh * 4 + x) for x in range(4)])
        for fo in range(16):
            wt, wk = C.wload(w_out[fo], 2048)
            bk, bkey = lhs_matmul_group(P, C, wt, wk, KC, lambda k: scr[:, YP + k, :], [("scr", YP + x) for x in range(16)])
            P.add("dve", lambda e, fo=fo, bk=bk: e.tensor_tensor(out=xt[:, fo, :], in0=xt[:, fo, :], in1=bk[:, :], op=ALU.add),
                  r=["xt", bkey], w=["xt"])
        C.rmsnorm(cst[:, G_FFN:G_FFN + 16], "f")
        for j in range(NFF):
            wt, wk = C.wload(w_gu[2 * j], 2048)
            bg, bgk = lhs_matmul_group(P, C, wt, wk, KC, lambda k: ht[:, k, :], ["ht"])
            wt, wk = C.wload(w_gu[2 * j + 1], 2048)
            bu, buk = lhs_matmul_group(P, C, wt, wk, KC, lambda k: ht[:, k, :], ["ht"])
            P.add("act", lambda e, bg=bg: e.activation(out=tmpg[:], in_=bg[:, :], func=AF.Silu), r=[bgk], w=["tmpg"])
            P.add("dve", lambda e, bu=bu, j=j: e.tensor_tensor(out=scr[:, j, :], in0=tmpg[:], in1=bu[:, :], op=ALU.mult),
                  r=["tmpg", buk], w=[("scr", j)])
        for fo in range(16):
            bk, bkey = C.nb()
            for pc in range(NP):
                wt, wk = C.wload(w_dn[fo * NP + pc], 1408)
                lhs_matmul_group(P, C, wt, wk, 11, lambda k: scr[:, k, :], [("scr", x) for x in range(pc * 11, pc * 11 + 11)],
                                 bk=bk, bkey=bkey, first=(pc == 0), last=(pc == NP - 1), kbase=pc * 11)
            P.add("dve", lambda e, fo=fo, bk=bk: e.tensor_tensor(out=xt[:, fo, :], in0=xt[:, fo, :], in1=bk[:, :], op=ALU.add),
                  r=["xt", bkey], w=["xt"])
        P.dma("sp", x2T_v[:, :, t0:t0 + T], xt[:], r=["xt"])
        C.rmsnorm(cst[:, G_ATT:G_ATT + 16], "a")
        P.dma("sp", posi[:], pos[0, t0:t0 + T].partition_broadcast(128), w=["posi"])
        P.add("dve", lambda e: e.tensor_copy(out=ang[:], in_=posi[:]), r=["posi"], w=["ang"])
        P.add("dve", lambda e: e.tensor_scalar(out=ang[:], in0=ang[:], scalar1=cst[:, IFQ:IFQ + 1], scalar2=None, op0=ALU.mult),
              r=["ang", "consts"], w=["ang"])
        def sin_of(dst, shift):
            P.add("dve", lambda e: e.tensor_scalar(out=tmpf[:], in0=ang[:], scalar1=shift, scalar2=1.0 / (2 * PI), op0=ALU.add, op1=ALU.mult),
                  r=["ang"], w=["tmpf"])
            P.add("dve", lambda e: e.tensor_copy(out=posi[:], in_=tmpf[:]), r=["tmpf"], w=["posi"])
            P.add("dve", lambda e: e.tensor_copy(out=tmpf[:], in_=posi[:]), r=["posi"], w=["tmpf"])
            P.add("dve", lambda e: e.tensor_scalar(out=tmpg[:], in0=ang[:], scalar1=shift, scalar2=None, op0=ALU.add), r=["ang"], w=["tmpg"])
            P.add("dve", lambda e: e.scalar_tensor_tensor(out=tmpg[:], in0=tmpf[:], scalar=-2 * PI, in1=tmpg[:], op0=ALU.mult, op1=ALU.add),
                  r=["tmpf", "tmpg"], w=["tmpg"])
            P.add("dve", lambda e: e.tensor_scalar(out=tmpf[:], in0=tmpg[:], scalar1=-PI, scalar2=1e9, op0=ALU.add, op1=ALU.mult),
                  r=["tmpg"], w=["tmpf"])
            P.add("dve", lambda e: e.tensor_scalar(out=tmpf[:], in0=tmpf[:], scalar1=0.0, scalar2=1.0, op0=ALU.max, op1=ALU.min),
                  r=["tmpf"], w=["tmpf"])
            P.add("dve", lambda e: e.scalar_tensor_tensor(out=tmpg[:], in0=tmpf[:], scalar=-2 * PI, in1=tmpg[:], op0=ALU.mult, op1=ALU.add),
                  r=["tmpf", "tmpg"], w=["tmpg"])
            P.add("dve", lambda e: e.tensor_scalar(out=tmpg[:], in0=tmpg[:], scalar1=-PI, scalar2=PI, op0=ALU.max, op1=ALU.min),
                  r=["tmpg"], w=["tmpg"])
            P.add("act", lambda e: e.activation(out=dst[:], in_=tmpg[:], func=AF.Sin), r=["tmpg"], w=["sincos"])
        sin_of(sinT, 0.0)
        P.add("dve", lambda e: e.tensor_scalar(out=sinT[:], in0=sinT[:], scalar1=cst[:, SGN:SGN + 1], scalar2=None, op0=ALU.mult),
              r=["sincos", "consts"], w=["sincos"])
        sin_of(cosT, 0.5 * PI)
        for c in range(32):
            wt, wk = C.wload(w_qk[c], 2048)
            bk, bkey = lhs_matmul_group(P, C, wt, wk, KC, lambda k: ht[:, k, :], ["ht"])
            sc = 0.125 if c < 16 else 1.0
            P.add("act", lambda e, bk=bk, sc=sc: e.activation(out=qsb[:], in_=bk[:, :], func=AF.Copy, scale=sc), r=[bkey], w=["qsb"])
            b2, b2k = C.nb()
            P.mm(b2[:, :], rot[:, :], qsb[:, :], True, True, r=["qsb", "consts"], w=[b2k])
            P.add("dve", lambda e: e.tensor_tensor(out=tmpf[:], in0=qsb[:], in1=cosT[:], op=ALU.mult), r=["qsb", "sincos"], w=["tmpf"])
            P.add("dve", lambda e, b2=b2: e.tensor_tensor(out=tmpg[:], in0=b2[:, :], in1=sinT[:], op=ALU.mult), r=[b2k, "sincos"], w=["tmpg"])
            o = qo[c % 2]
            ok = ("qo", c % 2)
            P.add("dve", lambda e, o=o: e.tensor_tensor(out=o[:], in0=tmpf[:], in1=tmpg[:], op=ALU.add), r=["tmpf", "tmpg"], w=[ok])
            dst = (qT if c < 16 else kT)[(c % 16) * 128:(c % 16 + 1) * 128, t0:t0 + T]
            P.dma("sp", dst, o[:], r=[ok])
        for n in range(4):
            for s in range(4):
                bk, bkey = C.nb()
                for q in range(4):
                    wt, wk = C.wload(w_v[n * 4 + q], 2048)
                    wv = wt[:, :].rearrange("p (k m) -> p k m", m=512)
                    for kk in range(4):
                        kc = 4 * q + kk
                        P.mm(bk[:, :], ht[:, kc, s * 128:(s + 1) * 128], wv[:, kk, :], kc == 0, kc == KC - 1, r=["ht", wk], w=[bkey])
                P.add("act", lambda e, bk=bk, s=s, n=n: e.activation(out=vsb[:, s, n * 512:(n + 1) * 512], in_=bk[:, :], func=AF.Copy),
                      r=[bkey], w=["vsb"])
        P.dma("sp", vtok[t0:t0 + T, :].rearrange("(s p) d -> p s d", p=128), vsb[:], r=["vsb"])
    P.emit()


def phase_b(sess, A, NTOK, S, lambda_init):
    nc = sess.nc
    NB2 = NTOK // T // 2
    W2 = NB2 * T
    NKT = S // 128
    KPM = W2 // 128
    qs, ks, vs, kg, vg, os_ = A["qs"], A["ks"], A["vs"], A["kg"], A["vg"], A["os"]
    lamv, gsub, diag, mcol, ident = A["lamv"], A["gsub"], A["diag"], A["mcol"], A["ident"]
    P = Prog(sess, "b_")
    _mk_eps(P)
    P.cc(lambda e: e.collective_compute("AllGather", ALU.bypass, replica_groups=[list(range(NCORES))], ins=[ks[:, :]], outs=[kg[:, :]]),
         w=["kg", "ccorder"])
    P.cc(lambda e: e.collective_compute("AllGather", ALU.bypass, replica_groups=[list(range(NCORES))], ins=[vs[:, :]], outs=[vg[:, :]]),
         w=["vg", "ccorder"])
    qts = [P.sb("qt%d" % i, [128, W2], BF16) for i in range(2)]
    kts = [P.sb("kt%d" % i, [128, S], BF16) for i in range(2)]
    vts = [P.sb("vt%d" % i, [128, NKT, 130], BF16) for i in range(2)]
    ot = P.sb("ot", [128, 4, 128], BF16)
    otT = P.sb("otT", [128, 512], BF16)
    E = [P.sb("E%d" % i, [128, 512], BF16) for i in range(4)]
    dg = P.sb("dg", [128, 4, 512], F32)
    mc = P.sb("mc", [128, 16], F32)
    mt = P.sb("mt", [128, 8, 4, 512], BF16)
    idb = P.sb("idb", [128, 128], BF16)
    lam4 = P.sb("lam4", [128, 4, 64], F32)
    gs_bc = P.sb("gs_bc", [128, 128], F32)
    sm = P.sb("sm", [128, 16], F32)
    o0 = P.sb("o0", [128, 128], F32)
    od = P.sb("od", [128, 128], F32)
    sq = P.sb("sq", [128, 128], F32)
    o0s = P.sb("o0s", [128, 4, 128], F32)
    psS = [P.ps("psS%d" % i, [128, 512], F32) for i in range(3)]
    acc = [P.ps("acc%d" % i, [128, 512], F32) for i in range(4)]
    pT = P.ps("pT", [128, 512], BF16)

    P.dma("sp", dg[:].rearrange("p a q -> p (a q)"), diag[:, :], w=["dg"])
    P.dma("sp", mc[:], mcol[:, :], w=["mc"])
    for m in range(8):
        P.add("dve", lambda e, m=m: e.tensor_scalar(out=mt[:, m, :, :], in0=dg[:], scalar1=mc[:, 8 + m:9 + m], scalar2=mc[:, m:m + 1],
                                                    op0=ALU.mult, op1=ALU.add), r=["dg", "mc"], w=["consts"])
    P.dma("pool", idb[:], ident[:, :], w=["consts"])
    P.dma("sp", lam4[:].rearrange("p a d -> p (a d)"), lamv.rearrange("a d -> (a d)").partition_broadcast(128), w=["lam4"])
    P.dma("sp", gs_bc[:], gsub[0, :].partition_broadcast(128), w=["gs"])
    P.add("dve", lambda e: e.tensor_scalar(out=gs_bc[:], in0=gs_bc[:], scalar1=1.0 - lambda_init, scalar2=None, op0=ALU.mult),
          r=["gs"], w=["consts"])
    P.add("dve", lambda e: e.tensor_tensor(out=o0[:, 0:64], in0=lam4[:, 0, :], in1=lam4[:, 1, :], op=ALU.mult), r=["lam4"], w=["o0"])
    P.add("dve", lambda e: e.reduce_sum(out=sm[:, 0:1], in_=o0[:, 0:64], axis=AX.X), r=["o0"], w=["sm"])
    P.add("dve", lambda e: e.tensor_tensor(out=o0[:, 64:128], in0=lam4[:, 2, :], in1=lam4[:, 3, :], op=ALU.mult), r=["lam4"], w=["o0"])
    P.add("dve", lambda e: e.reduce_sum(out=sm[:, 1:2], in_=o0[:, 64:128], axis=AX.X), r=["o0"], w=["sm"])
    P.add("act", lambda e: e.activation(out=sm[:, 0:2], in_=sm[:, 0:2], func=AF.Exp), r=["sm"], w=["sm"])
    P.add("dve", lambda e: e.tensor_tensor(out=sm[:, 2:3], in0=sm[:, 1:2], in1=sm[:, 0:1], op=ALU.subtract), r=["sm"], w=["sm"])
    P.add("dve", lambda e: e.tensor_scalar(out=sm[:, 3:4], in0=sm[:, 2:3], scalar1=-lambda_init, scalar2=None, op0=ALU.add),
          r=["sm"], w=["consts2"])
    for vt in vts:
        P.add("dve", lambda e, vt=vt: e.memset(vt[:, :, 128:129], 1.0), w=["vt1"])

    ei = 0
    si = 0
    LOOK = 2
    for b in range(2):
        for h in range(16):
            u = (b * 16 + h) % 2
            qt, kt, vt = qts[u], kts[u], vts[u]
            qk, kk_, vk = ("qt", u), ("kt", u), ("vt", u)
            P.dma("sp", qt[:], qs[h * 128:(h + 1) * 128, b * W2:(b + 1) * W2], w=[qk])
            for m in range(8):
                P.dma("sp", kt[:, m * W2:(m + 1) * W2], kg[m * D + h * 128:m * D + (h + 1) * 128, b * W2:(b + 1) * W2], r=["kg"], w=[kk_])
                P.dma("sp", vt[:, m * KPM:(m + 1) * KPM, 0:128],
                      vg[m * NTOK + b * W2:m * NTOK + (b + 1) * W2, h * 128:(h + 1) * 128].rearrange("(n p) d -> p n d", p=128),
                      r=["vg", "vt1"], w=[vk])
            for jj in range(NB2):
                tiles = [(jb, m, k4) for jb in range(jj + 1) for m in range(8) for k4 in range(4)]
                for c in range(2):
                    pend = {}
                    nt = len(tiles)
                    for step in range(nt + LOOK):
                        if step < nt:
                            jb, m, k4 = tiles[step]
                            ki = m * KPM + jb * 4 + k4
                            pS = psS[si % 3]
                            pk = ("psS", si % 3)
                            si += 1
                            P.mm(pS[:, :], kt[c * 64:(c + 1) * 64, ki * 128:(ki + 1) * 128],
                                 qt[c * 64:(c + 1) * 64, jj * 512:(jj + 1) * 512], True, True, r=[qk, kk_], w=[pk])
                            pend[step] = (pS, pk, ki, jb, m, k4)
                        idx = step - LOOK
                        if idx < 0:
                            continue
                        pS, pk, ki, jb, m, k4 = pend.pop(idx)
                        Et = E[ei % 4]
                        ek = ("E", ei % 4)
                        ei += 1
                        P.add("act", lambda e, Et=Et, pS=pS: e.activation(out=Et[:], in_=pS[:, :], func=AF.Exp), r=[pk], w=[ek])
                        if jb == jj:
                            P.add("dve", lambda e, Et=Et, m=m, k4=k4: e.tensor_tensor(out=Et[:], in0=Et[:], in1=mt[:, m, k4, :], op=ALU.mult),
                                  r=[ek, "consts"], w=[ek])
                        for qi in range(4):
                            P.mm(acc[qi][:, 0:129], Et[:, qi * 128:(qi + 1) * 128], vt[:, ki, 0:129],
                                 idx == 0, idx == nt - 1, r=[ek, vk, "vt1"], w=[("acc", qi)])
                    for qi in range(4):
                        a = acc[qi]
                        ak = ("acc", qi)
                        P.add("dve", lambda e, a=a: e.reciprocal(sm[:, 4:5], a[:, 128:129]), r=[ak], w=["sm"])
                        if c == 0:
                            P.add("act", lambda e, a=a, qi=qi: e.activation(out=o0s[:, qi, :], in_=a[:, 0:128], func=AF.Copy, scale=sm[:, 4:5]),
                                  r=[ak, "sm"], w=["o0s"])
                            continue
                        P.add("dve", lambda e: e.tensor_tensor(out=sm[:, 6:7], in0=sm[:, 4:5], in1=sm[:, 3:4], op=ALU.mult), r=["sm", "consts2"], w=["sm"])
                        P.add("dve", lambda e, a=a, qi=qi: e.scalar_tensor_tensor(out=od[:], in0=a[:, 0:128], scalar=sm[:, 6:7], in1=o0s[:, qi, :],
                                                                                  op0=ALU.mult, op1=ALU.add), r=[ak, "sm", "o0s"], w=["od"])
                        P.add("dve", lambda e: e.memset(sm[:, 7:8], 0.0), w=["sm"])
                        P.add("act", lambda e: e.activation(out=sq[:], in_=od[:], func=AF.Square, accum_out=sm[:, 7:8]), r=["od", "sm"], w=["sq", "sm"])
                        P.add("act", lambda e: e.activation(out=sm[:, 7:8], in_=sm[:, 7:8], func=AF.Sqrt, bias=EPS_AP[0][:, 0:1], scale=1.0 / 128),
                              r=["sm", "eps"], w=["sm"])
                        P.add("dve", lambda e: e.reciprocal(sm[:, 7:8], sm[:, 7:8]), r=["sm"], w=["sm"])
                        P.add("dve", lambda e, qi=qi: e.scalar_tensor_tensor(out=ot[:, qi, :], in0=od[:], scalar=sm[:, 7:8], in1=gs_bc[:],
                                                                             op0=ALU.mult, op1=ALU.mult), r=["od", "sm", "consts"], w=["ot"])
                for qi in range(4):
                    P.add("pe", lambda e, qi=qi: e.transpose(pT[:, qi * 128:(qi + 1) * 128], ot[:, qi, :], idb[:, :]), r=["ot", "consts"], w=["pT"])
                P.add("act", lambda e: e.activation(out=otT[:], in_=pT[:, :], func=AF.Copy), r=["pT"], w=["otT"])
                P.dma("pool", os_[h * 128:(h + 1) * 128, (b * NB2 + jj) * T:(b * NB2 + jj + 1) * T], otT[:], r=["otT"], w=["os"])
    P.emit()


def phase_c(sess, A, NTOK, NP):
    NFF = 11 * NP
    NT = NTOK // T
    nc = sess.nc
    xT, oT, cols, ident, w_r, w_o, w_gu, w_dn, outT = [A[k] for k in
        ("x2s", "os", "cols3", "ident", "w_r", "w_o", "w_gu3", "w_dn3", "outT")]
    P = Prog(sess, "c_")
    _mk_eps(P)
    C = Common(P, nc)
    xt, ht = C.xt, C.ht
    cst = P.sb("cst", [128, 32], F32)
    P.dma("sp", cst[:], cols[:, :], w=["consts"])
    G_MOE, G_FIN = 0, 16
    idf = P.sb("idf", [128, 128], F32)
    P.dma("sp", idf[:], ident[:, :], w=["consts"])
    wr = P.sb("wr", [128, 16, 8], BF16)
    P.dma("pool", wr[:].rearrange("p k e -> p (k e)"), w_r[:, :], w=["consts"])
    ob = P.sb("ob", [128, KC, T], BF16)
    scr = P.sb("scr", [128, max(NFF, 1), T], BF16)
    tmpg = P.sb("tmpg", [128, T], F32)
    tmpf = P.sb("tmpf", [128, T], F32)
    lg = P.sb("lg", [128, 8], F32)
    lg2 = P.sb("lg2", [128, 8], F32)
    sm = P.sb("sm", [128, 8], F32)
    gx = P.sb("gx", [128, 8, 128], F32)
    gbc = P.sb("gbc", [128, 8, T], F32)
    xT_v = xT.rearrange("(k p) t -> p k t", p=128)
    oT_v = oT.rearrange("(k p) t -> p k t", p=128)
    outT_v = outT.rearrange("(k p) t -> p k t", p=128)

    for it in range(NT):
        t0 = it * T
        P.dma("sp", xt[:], xT_v[:, :, t0:t0 + T], w=["xt"])
        P.dma("sp", ob[:], oT_v[:, :, t0:t0 + T], w=["ob"])
        for fo in range(16):
            wt, wk = C.wload(w_o[fo], 2048)
            bk, bkey = lhs_matmul_group(P, C, wt, wk, KC, lambda k: ob[:, k, :], ["ob"])
            P.add("dve", lambda e, fo=fo, bk=bk: e.tensor_tensor(out=xt[:, fo, :], in0=xt[:, fo, :], in1=bk[:, :], op=ALU.add),
                  r=["xt", bkey], w=["xt"])
        C.rmsnorm(cst[:, G_MOE:G_MOE + 16], "m")
        for s in range(4):
            bk, bkey = C.nb()
            for kc in range(KC):
                P.mm(bk[:, 0:8], ht[:, kc, s * 128:(s + 1) * 128], wr[:, kc, :], kc == 0, kc == KC - 1, r=["ht", "consts"], w=[bkey])
            P.add("dve", lambda e, bk=bk: e.tensor_copy(out=lg[:], in_=bk[:, 0:8]), r=[bkey], w=["lg"])
            P.add("dve", lambda e: e.reduce_max(out=sm[:, 0:1], in_=lg[:], axis=AX.X), r=["lg"], w=["sm"])
            P.add("dve", lambda e: e.tensor_scalar(out=lg2[:], in0=lg[:], scalar1=sm[:, 0:1], scalar2=1e9, op0=ALU.subtract, op1=ALU.mult),
                  r=["lg", "sm"], w=["lg2"])
            P.add("dve", lambda e: e.tensor_scalar(out=lg2[:], in0=lg2[:], scalar1=1.0, scalar2=0.0, op0=ALU.add, op1=ALU.max),
                  r=["lg2"], w=["lg2"])
            P.add("dve", lambda e: e.scalar_tensor_tensor(out=lg2[:], in0=lg2[:], scalar=-1e30, in1=lg[:], op0=ALU.mult, op1=ALU.add),
                  r=["lg2", "lg"], w=["lg2"])
            P.add("dve", lambda e: e.reduce_max(out=sm[:, 1:2], in_=lg2[:], axis=AX.X), r=["lg2"], w=["sm"])
            P.add("dve", lambda e: e.tensor_scalar(out=lg2[:], in0=lg[:], scalar1=sm[:, 1:2], scalar2=1e9, op0=ALU.subtract, op1=ALU.mult),
                  r=["lg", "sm"], w=["lg2"])
            P.add("dve", lambda e: e.tensor_scalar(out=lg2[:], in0=lg2[:], scalar1=1.0, scalar2=0.0, op0=ALU.add, op1=ALU.max),
                  r=["lg2"], w=["lg2"])
            P.add("dve", lambda e: e.tensor_scalar(out=lg2[:], in0=lg2[:], scalar1=1.0, scalar2=None, op0=ALU.min), r=["lg2"], w=["lg2"])
            P.add("dve", lambda e: e.tensor_scalar(out=sm[:, 2:3], in0=sm[:, 0:1], scalar1=-1.0, scalar2=None, op0=ALU.mult), r=["sm"], w=["sm"])
            P.add("act", lambda e: e.activation(out=lg[:], in_=lg[:], func=AF.Exp, bias=sm[:, 2:3], scale=1.0), r=["lg", "sm"], w=["lg"])
            P.add("dve", lambda e: e.tensor_tensor(out=lg[:], in0=lg[:], in1=lg2[:], op=ALU.mult), r=["lg", "lg2"], w=["lg"])
            P.add("dve", lambda e: e.reduce_sum(out=sm[:, 3:4], in_=lg[:], axis=AX.X), r=["lg"], w=["sm"])
            P.add("dve", lambda e: e.reciprocal(sm[:, 3:4], sm[:, 3:4]), r=["sm"], w=["sm"])
            P.add("dve", lambda e: e.tensor_scalar(out=lg[:], in0=lg[:], scalar1=sm[:, 3:4], scalar2=None, op0=ALU.mult), r=["lg", "sm"], w=["lg"])
            P.add("dve", lambda e: e.tensor_copy(out=gx[:], in_=lg[:].unsqueeze(2).to_broadcast([128, 8, 128])), r=["lg"], w=["gx"])
            for eh in range(2):
                b2, b2k = C.nb()
                for ee in range(4):
                    P.mm(b2[:, ee * 128:(ee + 1) * 128], gx[:, eh * 4 + ee, :], idf[:, :], True, True, r=["gx", "consts"], w=[b2k])
                P.add("act", lambda e, b2=b2, eh=eh, s=s: e.activation(out=gbc[:, eh * 4:eh * 4 + 4, s * 128:(s + 1) * 128],
                                                                      in_=b2[:, :].rearrange("p (g t) -> p g t", t=128), func=AF.Copy),
                      r=[b2k], w=["gbc"])
        for ex in range(8):
            for j in range(NFF):
                wt, wk = C.wload(w_gu[(ex * NFF + j) * 2], 2048)
                bg, bgk = lhs_matmul_group(P, C, wt, wk, KC, lambda k: ht[:, k, :], ["ht"])
                wt, wk = C.wload(w_gu[(ex * NFF + j) * 2 + 1], 2048)
                bu, buk = lhs_matmul_group(P, C, wt, wk, KC, lambda k: ht[:, k, :], ["ht"])
                P.add("act", lambda e, bg=bg: e.activation(out=tmpg[:], in_=bg[:, :], func=AF.Silu), r=[bgk], w=["tmpg"])
                P.add("dve", lambda e, bu=bu, j=j: e.tensor_tensor(out=scr[:, j, :], in0=tmpg[:], in1=bu[:, :], op=ALU.mult),
                      r=["tmpg", buk], w=[("scr", j)])
            for fo in range(16):
                bk, bkey = C.nb()
                for pc in range(NP):
                    wt, wk = C.wload(w_dn[(ex * 16 + fo) * NP + pc], 1408)
                    lhs_matmul_group(P, C, wt, wk, 11, lambda k: scr[:, k, :], [("scr", x) for x in range(pc * 11, pc * 11 + 11)],
                                     bk=bk, bkey=bkey, first=(pc == 0), last=(pc == NP - 1), kbase=pc * 11)
                P.add("dve", lambda e, bk=bk, ex=ex: e.tensor_tensor(out=tmpf[:], in0=bk[:, :], in1=gbc[:, ex, :], op=ALU.mult),
                      r=[bkey, "gbc"], w=["tmpf"])
                P.add("dve", lambda e, fo=fo: e.tensor_tensor(out=xt[:, fo, :], in0=xt[:, fo, :], in1=tmpf[:], op=ALU.add),
                      r=["xt", "tmpf"], w=["xt"])
        C.rmsnorm(cst[:, G_FIN:G_FIN + 16], "z")
        for kc in range(KC):
            P.add("dve", lambda e, kc=kc: e.scalar_tensor_tensor(out=xt[:, kc, :], in0=xt[:, kc, :], scalar=cst[:, G_FIN + kc:G_FIN + kc + 1],
                                                                   in1=C.rstd[:], op0=ALU.mult, op1=ALU.mult), r=["xt", "rstd", "consts"], w=["xt"])
        P.dma("sp", outT_v[:, :, t0:t0 + T], xt[:], r=["xt"])
    P.emit()


def build_fused(NTOK, NP, S):
    NFF = 11 * NP
    NB = NTOK // T
    nc = bass.Bass("TRN2", target_bir_lowering=False)
    A = {}

    def inp(name, shape, dt=F32):
        A[name] = nc.dram_tensor(name, list(shape), dt, kind="ExternalInput").ap()

    inp("xT", [D, NTOK]); inp("xh", [NB, D, 16]); inp("pos", [1, NTOK], I32); inp("invcnt", [1, NB * 64])
    inp("cols", [128, 64]); inp("lng", [1, 1024]); inp("lnb", [1, 1024]); inp("bsp", [1, 1024])
    inp("wsT", [128, 1024]); inp("tri", [128, 128]); inp("wpool", [128, 2048]); inp("rotP", [128, 128])
    inp("w_inA", [16, 128, 2048]); inp("w_inV", [8, 128, 2048]); inp("w_out", [16, 128, 2048])
    inp("w_gu", [2 * NFF, 128, 2048]); inp("w_dn", [16 * NP, 128, 1408]); inp("w_qk", [32, 128, 2048]); inp("w_v", [16, 128, 2048])
    inp("lamv", [4, 64]); inp("gsub", [1, 128]); inp("diag", [128, 2048]); inp("mcol", [128, 16]); inp("ident", [128, 128])
    inp("cols3", [128, 32]); inp("w_r", [128, 128]); inp("w_o", [16, 128, 2048])
    inp("w_gu3", [8 * 2 * NFF, 128, 2048]); inp("w_dn3", [8 * 16 * NP, 128, 1408])
    A["outT"] = nc.dram_tensor("outT", [D, NTOK], F32, kind="ExternalOutput").ap()
    A["x2s"] = nc.dram_tensor("x2s", [D, NTOK], F32).ap()
    A["qs"] = nc.dram_tensor("qs", [D, NTOK], BF16).ap()
    A["ks"] = nc.dram_tensor("ks", [D, NTOK], BF16).ap()
    A["vs"] = nc.dram_tensor("vs", [NTOK, D], BF16).ap()
    A["os"] = nc.dram_tensor("os", [D, NTOK], BF16).ap()
    A["kg"] = nc.dram_tensor("kg", [NCORES * D, NTOK], BF16, addr_space="Shared").ap()
    A["vg"] = nc.dram_tensor("vg", [NCORES * NTOK, D], BF16, addr_space="Shared").ap()
    lambda_init = 0.8 - 0.6 * math.exp(-0.3 * 1)
    sess = Sess(nc)
    phase_a(sess, A, NTOK, NP)
    phase_b(sess, A, NTOK, S, lambda_init)
    phase_c(sess, A, NTOK, NP)
    sess.close()
    return nc


def _lhs_chunks(W):
    K, N = W.shape
    return np.ascontiguousarray(W.reshape(K // 128, 128, N // 128, 128).transpose(2, 1, 0, 3)).reshape(N // 128, 128, (K // 128) * 128)


def _rhs_pieces(W):
    K, N = W.shape
    a = W.reshape(4, 4, 128, N // 512, 512).transpose(3, 0, 2, 1, 4)
    return np.ascontiguousarray(a).reshape((N // 512) * 4, 128, 2048)


def _dn_pieces(W, NP):
    a = W.reshape(NP, 11, 128, 16, 128).transpose(3, 0, 2, 1, 4)
    return np.ascontiguousarray(a).reshape(16 * NP, 128, 1408)


def _colpack(v):
    return np.ascontiguousarray(np.asarray(v, np.float32).reshape(-1, 128).T)


def kernel(x, positions,
           ev_norm_mix, ev_w_in, ev_w_pool, ev_pool_scale, ev_ln_g, ev_ln_b,
           ev_w_spatial, ev_b_spatial, ev_w_out, ev_norm_ffn, ev_w_gate, ev_w_up, ev_w_down,
           od_norm_attn, od_w_qkv, od_lam_q1, od_lam_k1, od_lam_q2, od_lam_k2, od_subln_g,
           od_w_o, od_norm_moe, od_w_router, od_we_gate, od_we_up, od_we_down,
           final_norm):
    f32 = np.float32
    x = np.asarray(x, f32)
    B, S, _ = x.shape
    assert B == 2
    DFF = np.asarray(ev_w_gate).shape[-1]
    NP = DFF // 1408
    NFF = 11 * NP
    NTOK = (B * S) // NCORES
    NB2 = NTOK // T // 2
    positions = np.asarray(positions, np.int32)
    A = lambda a: np.asarray(a, f32)

    w_in = A(ev_w_in)[0]
    cols = np.zeros((128, 64), f32)
    cols[:, 0:16] = _colpack(A(ev_norm_mix)[0])
    cols[:, 16:24] = _colpack(A(ev_pool_scale)[0])
    cols[:, 24:40] = _colpack(A(ev_norm_ffn)[0])
    cols[:, 40:56] = _colpack(A(od_norm_attn)[0])
    inv_freq = (10000.0 ** (-np.arange(0, 64, 2, dtype=f32) / 64)).astype(f32)
    cols[:, 56] = np.tile(inv_freq, 4)
    cols[:, 57] = np.tile(np.concatenate([-np.ones(32, f32), np.ones(32, f32)]), 2)
    tri = np.triu(np.ones((128, 128), f32))
    rotP = np.zeros((128, 128), f32)
    for m in range(128):
        k = (m // 64) * 64 + ((m % 64) + 32) % 64
        rotP[k, m] = 1.0
    wsT = np.ascontiguousarray(A(ev_w_spatial)[0].transpose(2, 0, 1)).reshape(128, 1024)
    wp = A(ev_w_pool)[0].reshape(4, 2, 128, 2, 128).transpose(2, 0, 1, 3, 4)
    wpool = np.ascontiguousarray(wp).reshape(128, 2048)
    wqkv = A(od_w_qkv)[0]
    wgu = np.empty((2 * NFF, 128, 2048), f32)
    wgu[0::2] = _lhs_chunks(A(ev_w_gate)[0])
    wgu[1::2] = _lhs_chunks(A(ev_w_up)[0])
    kk = np.arange(128)[:, None, None] + 128 * np.arange(4)[None, :, None]
    diag = (kk <= np.arange(512)[None, None, :]).astype(f32).reshape(128, 2048)
    cols3 = np.zeros((128, 32), f32)
    cols3[:, 0:16] = _colpack(A(od_norm_moe)[0])
    cols3[:, 16:32] = _colpack(A(final_norm))
    w_r = np.ascontiguousarray(A(od_w_router)[0].reshape(16, 128, 8).transpose(1, 0, 2)).reshape(128, 128)
    wgu3 = np.empty((8 * NFF * 2, 128, 2048), f32)
    wdn3 = np.empty((8 * 16 * NP, 128, 1408), f32)
    for e in range(8):
        wgu3[e * 2 * NFF:(e + 1) * 2 * NFF:2] = _lhs_chunks(A(od_we_gate)[0, e])
        wgu3[e * 2 * NFF + 1:(e + 1) * 2 * NFF:2] = _lhs_chunks(A(od_we_up)[0, e])
        wdn3[e * 16 * NP:(e + 1) * 16 * NP] = _dn_pieces(A(od_we_down)[0, e], NP)
    lamv = np.stack([A(od_lam_q1)[0], A(od_lam_k1)[0], A(od_lam_q2)[0], A(od_lam_k2)[0]], 0)
    shared = dict(
        cols=cols, lng=A(ev_ln_g)[0][None], lnb=A(ev_ln_b)[0][None], bsp=A(ev_b_spatial)[0].reshape(1, 1024),
        wsT=wsT, tri=tri, wpool=wpool, rotP=rotP,
        w_inA=np.concatenate([_lhs_chunks(w_in[:, 0:1024]), _lhs_chunks(w_in[:, 1024:2048])], 0),
        w_inV=_rhs_pieces(w_in[:, 2048:3072]), w_out=_lhs_chunks(A(ev_w_out)[0]), w_gu=wgu,
        w_dn=_dn_pieces(A(ev_w_down)[0], NP), w_qk=_lhs_chunks(wqkv[:, 0:4096]), w_v=_rhs_pieces(wqkv[:, 4096:6144]),
        lamv=lamv, gsub=A(od_subln_g)[0][None], diag=diag, ident=np.eye(128, dtype=f32),
        cols3=cols3, w_r=w_r, w_o=_lhs_chunks(A(od_w_o)[0]), w_gu3=wgu3, w_dn3=wdn3)
    in_maps = []
    for c in range(NCORES):
        xs, xhs, ps, ics = [], [], [], []
        for b in range(B):
            for jj in range(NB2):
                t0 = (8 * jj + c) * T
                xs.append(x[b, t0:t0 + T])
                xhs.append(x[b, t0 - 16:t0].T if t0 > 0 else np.zeros((D, 16), f32))
                ps.append(positions[b, t0:t0 + T])
                ic = np.zeros((4, 16), f32)
                for g, w in enumerate((2, 4, 8, 16)):
                    ic[g] = 1.0 / np.minimum(np.arange(16) + 1, w) if t0 == 0 else 1.0 / w
                ics.append(ic.reshape(64))
        mcol = np.zeros((128, 16), f32)
        mcol[:, 0:8] = (np.arange(8) < c).astype(f32)[None]
        mcol[:, 8 + c] = 1.0
        m = dict(shared)
        m.update(xT=np.ascontiguousarray(np.concatenate(xs, 0).T), xh=np.ascontiguousarray(np.stack(xhs, 0)),
                 pos=np.concatenate(ps)[None].copy(), invcnt=np.concatenate(ics)[None].copy(), mcol=mcol)
        in_maps.append(m)
    res = run_bass_kernel_spmd(build_fused(NTOK, NP, S), in_maps, core_ids=list(range(NCORES))).results
    out = np.empty((B, S, D), f32)
    for c in range(NCORES):
        o = res[c]["outT"].T
        i = 0
        for b in range(B):
            for jj in range(NB2):
                t0 = (8 * jj + c) * T
                out[b, t0:t0 + T] = o[i * T:(i + 1) * T]
                i += 1
    return out
```
